# Optimizing a Trainium2 kernel written in Bass

```python
import math
import jax, jax.numpy as jnp
from jax import lax
import numpy as np

D_MODEL = 1024
BATCH = 8
SEQ = 4096
DEPTH = 1

N_META = 16
EPS = 1e-6
ROPE_THETA = 10000.0

DA_HEADS = 4
DA_HEAD_DIM = D_MODEL // 16
DA_V_DIM = 2 * DA_HEAD_DIM
DA_QK_WIDTH = DA_HEADS * 2 * DA_HEAD_DIM
DA_V_WIDTH = DA_HEADS * DA_V_DIM
Q_BLOCK = 128

GLA_HEADS = 4
GLA_DK = D_MODEL // 16
GLA_DV = D_MODEL // 8
GLA_QK_WIDTH = GLA_HEADS * GLA_DK
GLA_V_WIDTH = GLA_HEADS * GLA_DV
GLA_GATE_RANK = 16
GLA_GATE_TAU = 16.0
GLA_CHUNK = 64

N_BRANCH = 2
BRANCH_WIDTH = DA_V_WIDTH

IN_SPLITS = (DA_QK_WIDTH, DA_QK_WIDTH, DA_V_WIDTH,
             GLA_QK_WIDTH, GLA_QK_WIDTH, GLA_V_WIDTH, GLA_V_WIDTH, GLA_GATE_RANK,
             N_BRANCH * D_MODEL)
D_IN = 2 * DA_QK_WIDTH + DA_V_WIDTH + 2 * GLA_QK_WIDTH + 2 * GLA_V_WIDTH + GLA_GATE_RANK + N_BRANCH * D_MODEL

N_GROUPS = 4
EXPERTS_PER_GROUP = 8
N_EXPERTS = N_GROUPS * EXPERTS_PER_GROUP
TOP_K = 2
D_EXPERT = D_MODEL // 2
MOE_BLOCK = 128

kernel_name = 'hybrid_diffattn_gla_hier_moe'


def rms_norm(x, gain):
    xf = x.astype(jnp.float32)
    y = xf * lax.rsqrt(jnp.mean(xf * xf, axis=-1, keepdims=True) + EPS)
    return (y * gain.astype(jnp.float32)).astype(x.dtype)


def apply_rope(x, pos):
    d = x.shape[-1]
    half = d // 2
    inv_freq = jnp.power(ROPE_THETA, -jnp.arange(half, dtype=jnp.float32) * 2.0 / d)
    ang = pos[:, None] * inv_freq[None, :]
    bshape = (pos.shape[0],) + (1,) * (x.ndim - 3) + (half,)
    cos = jnp.cos(ang).reshape(bshape).astype(x.dtype)
    sin = jnp.sin(ang).reshape(bshape).astype(x.dtype)
    x1, x2 = x[..., :half], x[..., half:]
    return jnp.concatenate([x1 * cos - x2 * sin, x2 * cos + x1 * sin], axis=-1)


def pad_time(x, left, right):
    pw = [(0, 0)] * x.ndim
    pw[1] = (left, right)
    return jnp.pad(x, pw)


def split_columns(z):
    outs = []
    off = 0
    for s in IN_SPLITS:
        outs.append(z[..., off:off + s])
        off += s
    return outs


def diff_attention(q, k, v, g_q, g_k, g_subln, lam_q1, lam_k1, lam_q2, lam_k2, layer_idx):
    bsz, length, _ = q.shape
    q = rms_norm(q.reshape(bsz, length, DA_HEADS, 2, DA_HEAD_DIM), g_q)
    k = rms_norm(k.reshape(bsz, length, DA_HEADS, 2, DA_HEAD_DIM), g_k)
    pos = jnp.arange(length, dtype=jnp.float32)
    q = apply_rope(q, pos)
    k = apply_rope(k, pos)
    n_blocks = -(-length // Q_BLOCK)
    lp = n_blocks * Q_BLOCK
    q = pad_time(q, 0, lp - length).transpose(0, 2, 3, 1, 4)
    k = pad_time(k, 0, lp - length).transpose(0, 2, 3, 1, 4)
    v = pad_time(v.reshape(bsz, length, DA_HEADS, DA_V_DIM), 0, lp - length).transpose(0, 2, 1, 3)
    lam_init = 0.8 - 0.6 * math.exp(-0.3 * layer_idx)
    lam = (jnp.exp(jnp.sum(lam_q1.astype(jnp.float32) * lam_k1.astype(jnp.float32)))
           - jnp.exp(jnp.sum(lam_q2.astype(jnp.float32) * lam_k2.astype(jnp.float32))) + lam_init)
    scale = DA_HEAD_DIM ** -0.5
    k_pos = jnp.arange(lp)

    def one_block(i):
        q_blk = lax.dynamic_slice_in_dim(q, i * Q_BLOCK, Q_BLOCK, axis=3)
        s = jnp.einsum('bhmqd,bhmkd->bhmqk', q_blk, k, preferred_element_type=jnp.float32) * scale
        q_pos = i * Q_BLOCK + jnp.arange(Q_BLOCK)
        s = jnp.where(k_pos[None, :] <= q_pos[:, None], s, -jnp.inf)
        p = jax.nn.softmax(s, axis=-1)
        a = p[:, :, 0] - lam * p[:, :, 1]
        return jnp.einsum('bhqk,bhke->bhqe', a.astype(v.dtype), v)

    o = lax.map(one_block, jnp.arange(n_blocks))
    o = o.transpose(1, 0, 3, 2, 4).reshape(bsz, lp, DA_HEADS, DA_V_DIM)[:, :length]
    o = rms_norm(o, g_subln) * (1.0 - lam_init)
    return o.reshape(bsz, length, DA_V_WIDTH)


def gated_linear_attention(q, k, v, r, g_lr, w_gate_up, b_gate, g_norm):
    bsz, length, _ = q.shape
    f32 = jnp.float32
    q = q.astype(f32).reshape(bsz, length, GLA_HEADS, GLA_DK) * (GLA_DK ** -0.5)
    k = k.astype(f32).reshape(bsz, length, GLA_HEADS, GLA_DK)
    v = v.astype(f32).reshape(bsz, length, GLA_HEADS, GLA_DV)
    log_a = jax.nn.log_sigmoid(jnp.einsum('blr,rk->blk', g_lr, w_gate_up, preferred_element_type=f32)
                               + b_gate.astype(f32)) / GLA_GATE_TAU
    log_a = log_a.reshape(bsz, length, GLA_HEADS, GLA_DK)
    left = (-N_META) % GLA_CHUNK
    right = (-(length + left)) % GLA_CHUNK
    n_chunks = (length + left + right) // GLA_CHUNK

    def to_chunks(t):
        t = pad_time(t, left, right)
        return t.reshape(bsz, n_chunks, GLA_CHUNK, GLA_HEADS, t.shape[-1]).transpose(1, 0, 3, 2, 4)

    causal = jnp.tril(jnp.ones((GLA_CHUNK, GLA_CHUNK), dtype=bool))

    def chunk_step(state, inp):
        q_c, k_c, v_c, a_c = inp
        b = jnp.cumsum(a_c, axis=2)
        b_last = b[:, :, -1:, :]
        inter = jnp.einsum('bhtd,bhde->bhte', q_c * jnp.exp(b), state)
        rel = jnp.where(causal[:, :, None], b[:, :, :, None, :] - b[:, :, None, :, :], -jnp.inf)
        scores = jnp.einsum('bhtd,bhsd,bhtsd->bhts', q_c, k_c, jnp.exp(rel))
        o = inter + jnp.einsum('bhts,bhse->bhte', scores, v_c)
        state = (jnp.exp(b_last[:, :, 0, :])[..., None] * state
                 + jnp.einsum('bhsd,bhse->bhde', k_c * jnp.exp(b_last - b), v_c))
        return state, o

    state0 = jnp.zeros((bsz, GLA_HEADS, GLA_DK, GLA_DV), f32)
    _, o = lax.scan(chunk_step, state0, (to_chunks(q), to_chunks(k), to_chunks(v), to_chunks(log_a)))
    o = o.transpose(1, 0, 3, 2, 4).reshape(bsz, n_chunks * GLA_CHUNK, GLA_HEADS, GLA_DV)[:, left:left + length]
    o = rms_norm(o, g_norm).reshape(bsz, length, GLA_V_WIDTH) * jax.nn.silu(r.astype(f32))
    return o.astype(r.dtype)


def hierarchical_moe(h, w_rg, b_rg, w_re, b_re, w_g, w_u, w_d):
    bsz, length, d = h.shape
    n = bsz * length
    t = h.reshape(n, d)
    g_logits = jnp.einsum('nd,dg->ng', t, w_rg, preferred_element_type=jnp.float32) + b_rg.astype(jnp.float32)
    g_prob = jax.nn.softmax(g_logits, axis=-1)
    g_idx = jnp.argmax(g_logits, axis=-1)
    g_w = jnp.take_along_axis(g_prob, g_idx[:, None], axis=1)[:, 0]
    e_logits_all = jnp.einsum('nd,dge->nge', t, w_re, preferred_element_type=jnp.float32) + b_re.astype(jnp.float32)
    e_logits = jnp.take_along_axis(e_logits_all, g_idx[:, None, None], axis=1)[:, 0]
    top_v, top_i = lax.top_k(e_logits, TOP_K)
    e_w = jax.nn.softmax(top_v, axis=-1)
    ids = (g_idx[:, None] * EXPERTS_PER_GROUP + top_i).reshape(-1).astype(jnp.int32)
    wts = (g_w[:, None] * e_w).reshape(-1)
    tok = jnp.repeat(jnp.arange(n, dtype=jnp.int32), TOP_K)
    order = jnp.argsort(ids)
    ids_s, tok_s, w_s = ids[order], tok[order], wts[order]
    counts = jnp.bincount(ids, length=N_EXPERTS)
    starts = jnp.cumsum(counts) - counts
    padded = (counts + MOE_BLOCK - 1) // MOE_BLOCK * MOE_BLOCK
    pends = jnp.cumsum(padded)
    pstarts = pends - padded
    dest = pstarts[ids_s] + jnp.arange(n * TOP_K) - starts[ids_s]
    n_slots = -(-(n * TOP_K) // MOE_BLOCK) * MOE_BLOCK + N_EXPERTS * MOE_BLOCK
    n_blocks = n_slots // MOE_BLOCK
    slot_tok = jnp.full((n_slots,), n, jnp.int32).at[dest].set(tok_s)
    slot_w = jnp.zeros((n_slots,), jnp.float32).at[dest].set(w_s)
    block_e = jnp.minimum(jnp.searchsorted(pends, jnp.arange(n_blocks) * MOE_BLOCK, side='right'), N_EXPERTS - 1)
    t_pad = jnp.concatenate([t, jnp.zeros((1, d), t.dtype)], axis=0)

    def run_block(args):
        tok_b, w_b, e = args
        xb = t_pad[tok_b]
        hid = jax.nn.silu(xb @ w_g[e]) * (xb @ w_u[e])
        return (hid @ w_d[e]) * w_b[:, None].astype(xb.dtype)

    y = lax.map(run_block, (slot_tok.reshape(n_blocks, MOE_BLOCK), slot_w.reshape(n_blocks, MOE_BLOCK), block_e))
    out = jnp.zeros((n + 1, d), h.dtype).at[slot_tok].add(y.reshape(n_slots, d).astype(h.dtype))
    return out[:n].reshape(bsz, length, d)


def setup_inputs(seed: int = 0) -> dict:
    key = jax.random.key(seed)
    ks = jax.random.split(key, 26)
    f32 = jnp.float32

    def nrm(k, shape, scale):
        return jax.random.normal(k, shape, f32) * scale

    return {
        'x': nrm(ks[0], (BATCH, SEQ, D_MODEL), 1.0),
        'meta_tokens': nrm(ks[1], (N_META, D_MODEL), 1.0),
        'g_mix_norm': 1.0 + nrm(ks[2], (DEPTH, D_MODEL), 0.02),
        'w_in': nrm(ks[3], (DEPTH, D_MODEL, D_IN), D_MODEL ** -0.5),
        'g_q_norm': 1.0 + nrm(ks[4], (DEPTH, DA_HEAD_DIM), 0.02),
        'g_k_norm': 1.0 + nrm(ks[5], (DEPTH, DA_HEAD_DIM), 0.02),
        'lambda_q1': nrm(ks[6], (DEPTH, DA_HEAD_DIM), 0.1),
        'lambda_k1': nrm(ks[7], (DEPTH, DA_HEAD_DIM), 0.1),
        'lambda_q2': nrm(ks[8], (DEPTH, DA_HEAD_DIM), 0.1),
        'lambda_k2': nrm(ks[9], (DEPTH, DA_HEAD_DIM), 0.1),
        'g_diff_subln': 1.0 + nrm(ks[10], (DEPTH, DA_V_DIM), 0.02),
        'w_gla_gate_up': nrm(ks[11], (DEPTH, GLA_GATE_RANK, GLA_QK_WIDTH), GLA_GATE_RANK ** -0.5),
        'b_gla_gate': nrm(ks[12], (DEPTH, GLA_QK_WIDTH), 0.1),
        'g_gla_norm': 1.0 + nrm(ks[13], (DEPTH, GLA_DV), 0.02),
        'w_branch': nrm(ks[14], (DEPTH, N_BRANCH, BRANCH_WIDTH, D_MODEL), BRANCH_WIDTH ** -0.5),
        'b_merge_gate': nrm(ks[15], (DEPTH, N_BRANCH, D_MODEL), 0.1),
        'w_out': nrm(ks[16], (DEPTH, D_MODEL, D_MODEL), D_MODEL ** -0.5),
        'g_ffn_norm': 1.0 + nrm(ks[17], (DEPTH, D_MODEL), 0.02),
        'w_router_group': nrm(ks[18], (DEPTH, D_MODEL, N_GROUPS), D_MODEL ** -0.5),
        'b_router_group': nrm(ks[19], (DEPTH, N_GROUPS), 0.01),
        'w_router_expert': nrm(ks[20], (DEPTH, D_MODEL, N_GROUPS, EXPERTS_PER_GROUP), D_MODEL ** -0.5),
        'b_router_expert': nrm(ks[21], (DEPTH, N_GROUPS, EXPERTS_PER_GROUP), 0.01),
        'w_exp_gate': nrm(ks[22], (DEPTH, N_EXPERTS, D_MODEL, D_EXPERT), D_MODEL ** -0.5),
        'w_exp_up': nrm(ks[23], (DEPTH, N_EXPERTS, D_MODEL, D_EXPERT), D_MODEL ** -0.5),
        'w_exp_down': nrm(ks[24], (DEPTH, N_EXPERTS, D_EXPERT, D_MODEL), D_EXPERT ** -0.5),
    }


def reference(x, meta_tokens, g_mix_norm, w_in, g_q_norm, g_k_norm, lambda_q1, lambda_k1, lambda_q2, lambda_k2,
              g_diff_subln, w_gla_gate_up, b_gla_gate, g_gla_norm, w_branch, b_merge_gate, w_out, g_ffn_norm,
              w_router_group, b_router_group, w_router_expert, b_router_expert, w_exp_gate, w_exp_up, w_exp_down):
    bsz = x.shape[0]
    meta = jnp.broadcast_to(meta_tokens.astype(x.dtype)[None], (bsz, N_META, D_MODEL))
    u = jnp.concatenate([meta, x], axis=1)
    length = u.shape[1]
    for l in range(DEPTH):
        h = rms_norm(u, g_mix_norm[l])
        z = jnp.einsum('bld,de->ble', h, w_in[l])
        da_q, da_k, da_v, gla_q, gla_k, gla_v, gla_r, gla_g, gate_logits = split_columns(z)
        o_a = diff_attention(da_q, da_k, da_v, g_q_norm[l], g_k_norm[l], g_diff_subln[l],
                             lambda_q1[l], lambda_k1[l], lambda_q2[l], lambda_k2[l], l)
        o_b = gated_linear_attention(gla_q, gla_k, gla_v, gla_r, gla_g, w_gla_gate_up[l], b_gla_gate[l], g_gla_norm[l])
        branches = jnp.stack([o_a, o_b], axis=2)
        y = jnp.einsum('blnw,nwd->blnd', branches, w_branch[l])
        gates = jax.nn.sigmoid(gate_logits.reshape(bsz, length, N_BRANCH, D_MODEL) + b_merge_gate[l])
        merged = jnp.sum(gates * y, axis=2)
        u = u + jnp.einsum('bld,de->ble', merged, w_out[l])
        h2 = rms_norm(u, g_ffn_norm[l])
        u = u + hierarchical_moe(h2, w_router_group[l], b_router_group[l], w_router_expert[l], b_router_expert[l],
                                 w_exp_gate[l], w_exp_up[l], w_exp_down[l])
    return u[:, N_META:]
```

```python
import math
import numpy as np
import ml_dtypes
from contextlib import ExitStack
import concourse.bass as bass
import concourse.mybir as mybir
from concourse.bass_utils import run_bass_kernel_spmd

F32 = mybir.dt.float32
BF16 = mybir.dt.bfloat16
I32 = mybir.dt.int32
AF = mybir.ActivationFunctionType
ALU = mybir.AluOpType
AX = mybir.AxisListType

ND_SEM = 20
COMPUTE = ('pe', 'act', 'dve', 'pool')
EPS = 1e-6
N_META = 16


class Tok:
    __slots__ = ('name', 'w', 'r', 'ps')

    def __init__(self, name=''):
        self.name = name
        self.ps = False
        self.w = None
        self.r = []


class Tile:
    def __init__(self, t, name, toks=None):
        self.t = t
        self.toks = toks if toks is not None else [Tok(name)]

    def __getitem__(self, k):
        return self.t[k]


def _toks(xs):
    out = []
    for x in xs:
        if isinstance(x, Tile):
            out.extend(x.toks)
        elif isinstance(x, Tok):
            out.append(x)
        else:
            out.extend(_toks(x))
    return tuple(out)


class Prog:
    def __init__(self, nc):
        self.nc = nc
        self.ops = []
        self.costs = []
        self.reorder_on = True
        self.trace_sim = None
        self.use_blevel = True
        self.marks = []
        self.es = ExitStack()
        self.n_alloc = 0

    def sb(self, shape, dt, name=None):
        self.n_alloc += 1
        name = "s_" + (name or f"sb{self.n_alloc}")
        t = self.es.enter_context(self.nc.sbuf_tensor(name, list(shape), dt))
        return Tile(t, name)

    def ps(self, shape, dt, name=None):
        self.n_alloc += 1
        name = name or f"ps{self.n_alloc}"
        t = self.es.enter_context(self.nc.psum_tensor(name, list(shape), dt))
        tl = Tile(t, name)
        tl.toks[0].ps = True
        return tl

    def dram(self, name, shape, dt, kind="Internal"):
        t = self.nc.dram_tensor(name, list(shape), dt, kind=kind)
        return Tile(t, name)

    def op(self, eng, fn, reads=(), writes=(), cost=0.3):
        r, w = _toks(reads), _toks(writes)
        w = w + tuple(t for t in r if t.ps)
        r = tuple(t for t in r if not t.ps)
        self.ops.append((eng, fn, r, w, False))
        self.costs.append(cost)

    def dma(self, q, fn, reads=(), writes=(), nbytes=65536):
        self.ops.append((q, fn, _toks(reads), _toks(writes), True))
        self.costs.append(nbytes)

    def reorder(self, deps):
        import heapq
        ops, costs = self.ops, self.costs
        n = len(ops)
        engs = ['pe', 'act', 'dve', 'pool', 'sp']
        succ = [[] for _ in range(n)]
        indeg = [0] * n
        for i in range(n):
            indeg[i] = len(deps[i])
            for j in deps[i]:
                succ[j].append(i)
        per = {e: [] for e in engs}
        for i, o in enumerate(ops):
            per[o[0]].append(i)
        dur_est = [(costs[i] / 250e3 + 2.0) if ops[i][4] else costs[i] for i in range(n)]
        blevel = [0.0] * n
        for i in range(n - 1, -1, -1):
            m_ = 0.0
            for k in succ[i]:
                if blevel[k] > m_:
                    m_ = blevel[k]
            blevel[i] = dur_est[i] + m_
        ptr = {e: 0 for e in engs}
        done = [False] * n
        ready = [0.0] * n
        te = {e: 0.0 for e in engs}
        dma_free = 0.0
        qfree = {e: 0.0 for e in engs}
        order = {e: [] for e in engs}
        W = {'pe': 256, 'act': 64, 'dve': 64, 'pool': 16, 'sp': 24}
        left = n
        BW = 180e3
        while left:
            best = None
            for e in engs:
                lst = per[e]
                p = ptr[e]
                while p < len(lst) and done[lst[p]]:
                    p += 1
                ptr[e] = p
                seen = 0
                q = p
                while q < len(lst) and seen < W[e]:
                    i = lst[q]
                    q += 1
                    if done[i]:
                        continue
                    seen += 1
                    if indeg[i]:
                        continue
                    st_ = max(te[e], ready[i])
                    key = (st_, -blevel[i] if self.use_blevel else i, i)
                    if best is None or key < best[0]:
                        best = (key, e, i)
                    if st_ <= te[e] and not self.use_blevel:
                        break
            assert best is not None, "scheduler deadlock"
            (st_, _, _), e, i = best
            te_before = te[e]
            if ops[i][4]:
                issue = 0.9 if e == 'pool' else 0.12
                t0 = max(st_ + issue, dma_free, qfree[e])
                dur = costs[i] / (215e3 if e == 'pool' else 300e3)
                qfree[e] = t0 + dur
                dma_free = t0 + costs[i] / 340e3
                fin = t0 + dur + 1.8
                te[e] = st_ + issue
            else:
                te[e] = st_ + costs[i]
                fin = te[e] + 0.12
            done[i] = True
            left -= 1
            order[e].append(i)
            if self.trace_sim is not None:
                self.trace_sim.append((i, e, st_, fin, te_before, ready[i]))
            for k in succ[i]:
                indeg[k] -= 1
                if ready[k] < fin:
                    ready[k] = fin
        self.sim_time = max(te.values())
        return order

    def build(self):
        nc = self.nc
        ops = self.ops
        n = len(ops)
        deps = [None] * n
        for i, (eng, fn, reads, writes, isdma) in enumerate(ops):
            d = set()
            for b in reads:
                if b.w is not None:
                    d.add(b.w)
            for b in writes:
                if b.w is not None:
                    d.add(b.w)
                d.update(b.r)
            d.discard(i)
            deps[i] = d
            for b in reads:
                b.r.append(i)
            for b in writes:
                b.w = i
                b.r = []
        engs = ['pe', 'act', 'dve', 'pool', 'sp']
        self.deps_saved = deps
        if self.reorder_on:
            stream = self.reorder(deps)
        else:
            stream = {e: [] for e in engs}
            for i, o in enumerate(ops):
                stream[o[0]].append(i)
        pos = [0] * n
        for e in engs:
            for p_, i in enumerate(stream[e]):
                pos[i] = p_
        needed = [False] * n
        real = [None] * n
        for i in range(n):
            e = ops[i][0]
            rd = []
            for j in deps[i]:
                ej, isd = ops[j][0], ops[j][4]
                if not isd and ej == e:
                    if e in ('pe', 'sp'):
                        continue
                    assert pos[i] > pos[j]
                rd.append(j)
                needed[j] = True
            real[i] = rd
        cnt = [0] * n
        dmaidx = [0] * n
        ccount = {e: 0 for e in engs}
        dcount = {e: 0 for e in engs}
        for e in engs:
            for i in stream[e]:
                if ops[i][4]:
                    dmaidx[i] = dcount[e]
                    dcount[e] += 1
                elif needed[i]:
                    ccount[e] += 1
                    cnt[i] = ccount[e]
        es = self.es
        csem = {e: es.enter_context(nc.semaphore(f"c_{e}")) for e in COMPUTE}
        dsem = {}
        for e in engs:
            if dcount[e]:
                dsem[e] = [es.enter_context(nc.semaphore(f"d_{e}{k}"))
                           for k in range(min(ND_SEM, dcount[e]))]
        self.stats = dict(n_ops=n, per_eng={e: len(stream[e]) for e in engs},
                          incs=dict(ccount), dmas=dict(dcount))

        def dma_sem_val(j):
            e = ops[j][0]
            k = dmaidx[j]
            return dsem[e][k % ND_SEM], 16 * (k // ND_SEM + 1)

        block = es.enter_context(nc.Block())
        nwaits = {}

        def make(e):
            def body(engine):
                waited = {}
                nw = 0
                for i in stream[e]:
                    _, fn, _, _, isdma = ops[i]
                    need = {}
                    for j in real[i]:
                        if ops[j][4]:
                            s, v = dma_sem_val(j)
                        else:
                            s, v = csem[ops[j][0]], cnt[j]
                        key = id(s)
                        if key not in need or need[key][1] < v:
                            need[key] = (s, v)
                    if isdma:
                        k = dmaidx[i]
                        if k >= ND_SEM:
                            s = dsem[e][k % ND_SEM]
                            v = 16 * (k // ND_SEM)
                            key = id(s)
                            if key not in need or need[key][1] < v:
                                need[key] = (s, v)
                    for key, (s, v) in need.items():
                        if waited.get(key, 0) >= v:
                            continue
                        waited[key] = v
                        engine.wait_ge(s, v)
                        nw += 1
                    ins = fn(engine)
                    if isdma:
                        s, _ = dma_sem_val(i)
                        ins.then_inc(s, 16)
                    elif needed[i]:
                        ins.then_inc(csem[e], 1)
                if e == 'sp':
                    for q in engs:
                        if dcount[q]:
                            for k in range(max(0, dcount[q] - ND_SEM), dcount[q]):
                                engine.wait_ge(dsem[q][k % ND_SEM], 16 * (k // ND_SEM + 1))
                nwaits[e] = nw
            return body

        block.sync(make('sp'))
        if stream['pe']:
            block.tensor(make('pe'))
        if stream['act']:
            block.scalar(make('act'))
        if stream['dve']:
            block.vector(make('dve'))
        if stream['pool']:
            block.gpsimd(make('pool'))
        self.stats['waits'] = nwaits
        self.stats['sim_us'] = getattr(self, 'sim_time', None)
        es.close()
        return nc


class Rot:
    def __init__(self, tiles):
        self.tiles = tiles
        self.i = 0

    def __call__(self):
        t = self.tiles[self.i % len(self.tiles)]
        self.i += 1
        return t


def build_program(T, CAP, dbg=False):
    assert T % 512 == 0 and CAP % 128 == 0
    NG = T // 512
    NT = T // 128
    NB = CAP // 128
    NSLOT = 32 * CAP
    LAM_INIT = 0.8 - 0.6 * math.exp(-0.3 * 0)
    nc = bass.Bass("TRN2", target_bir_lowering=False)
    P = Prog(nc)

    def din(name, shape, dt=F32):
        return P.dram(name, shape, dt, kind="ExternalInput")
    x_d = din("x", [T, 1024])
    meta_d = din("meta", [16, 1024])
    wA_d = din("wA", [15, 128, 4096])
    wE_d = din("wE", [96, 128, 4096])
    wr_d = din("wr", [128, 8 * 36])
    rope_d = din("rope", [T + 16, 96])
    cm_d = din("cmask", [128, 6 * 128])
    gmixT_d = din("gmixT", [128, 8])
    bmgT_d = din("bmgT", [128, 16])
    vec_d = din("vecs", [1, 1956])
    wup_d = din("wup", [16, 256])
    ebase_d = din("ebase", [1, 32])
    out_d = P.dram("out", [T, 1024], F32, kind="ExternalOutput")
    Xg_d = P.dram("Xg", [NSLOT + 128, 1024], BF16)
    Yg_d = P.dram("Yg", [NSLOT + 128, 1024], F32)
    U_d = P.dram("U", [T, 1024], F32)
    dbg_d = {}
    if dbg:
        for nm, shp in (("d_oa", [T, 512]), ("d_ob", [T, 512]), ("d_u", [T, 1024]), ("d_lg", [T, 36]),
                        ("d_sl", [T, 4]), ("d_st", [128, 8]), ("d_o", [128, 128]), ("d_acc", [128, 258])):
            dbg_d[nm] = P.dram(nm, shp, F32, kind="ExternalOutput")

    V_GFFN, V_GQ, V_GK, V_GSUB, V_GGLA, V_BGATE, V_BR, V_LAM = 0, 1024, 1088, 1152, 1280, 1408, 1664, 1700

    vecs = P.sb([128, 1956], F32, "vecs")
    cm = P.sb([128, 6 * 128], F32, "cm")
    ident_f = cm[:, 0:128]
    tri01_f = cm[:, 128:256]
    slt01_f = cm[:, 256:384]
    triS_f = cm[:, 512:640]
    sgtS_f = cm[:, 640:768]
    cmb = P.sb([128, 4 * 128], BF16, "cmb")
    ident_b = cmb[:, 0:128]
    maskb_b = cmb[:, 384:512]
    ones_f = P.sb([128, 128], F32, "ones_f")
    gmixT = P.sb([128, 8], F32, "gmixT")
    bmgT = P.sb([128, 16], F32, "bmgT")
    nbmgT = P.sb([128, 16], F32, "nbmgT")
    wr_sb = P.sb([128, 8 * 36], F32, "wr_sb")
    wup_f = P.sb([16, 256], F32, "wup_f")
    wup_b = P.sb([16, 256], BF16, "wup_b")
    gsub_s = P.sb([128, 128], F32, "gsub_s")
    neglam = P.sb([128, 1], F32, "neglam")
    lamt = P.sb([128, 4], F32, "lamt")
    junk_f = P.sb([128, 512], F32, "junk_f")
    junk_b = P.sb([128, 1024], BF16, "junk_b")
    junk_d = P.sb([128, 64], F32, "junk_d")
    junk_d2 = P.sb([128, 128], F32, "junk_d2")
    cntb = P.sb([128, 32], F32, "cntb")
    limb = P.sb([128, 32], F32, "limb")
    trashc = P.sb([128, 1], F32, "trashc")

    AR_KT = 4 * (T + 16)
    AR_VA = (NT + 1) * 4 * 129
    arena = P.sb([128, AR_KT + AR_VA], BF16, "arena")
    KT = {}
    VA = {}
    off = 0
    for g in [-1] + list(range(NG)):
        ntok = 16 if g < 0 else 512
        KT[g] = Tile(arena[:, off:off + 4 * ntok].rearrange("p (h t) -> p h t", h=4), f"KT{g}")
        off += 4 * ntok
    for g in [-1] + list(range(NG)):
        ntl = 1 if g < 0 else 4
        VA[g] = Tile(arena[:, off:off + ntl * 516].rearrange("p (j h e) -> p j h e", j=ntl, h=4), f"VA{g}")
        off += ntl * 516
    arena_toks = [t for g in KT for t in KT[g].toks] + [t for g in VA for t in VA[g].toks]

    NWB = 4
    wbufs = [P.sb([128, 4096], BF16, f"wb{i}") for i in range(NWB)]
    hT = P.sb([128, 8, 512], BF16, "hT")
    frow = Rot([P.sb([128, 1024], F32, f"frow{i}") for i in range(5)])
    ropet = P.sb([128, 4, 96], F32, "ropet")
    scr8 = P.sb([128, 4096], BF16, "scr8")
    QT = Tile(scr8[:, :].rearrange("p (h m t) -> p h m t", h=4, m=2), "QT")
    scrG = P.sb([128, 2048], BF16, "scrG")
    qtT = Tile(scrG[:, 0:1024].rearrange("p (a t) -> p a t", a=2), "qtT")
    ktT = Tile(scrG[:, 1024:2048].rearrange("p (a t) -> p a t", a=2), "ktT")
    mT = Tile(scr8[:, :].rearrange("p (c t) -> p c t", c=8), "mT", toks=QT.toks)
    gT = P.sb([16, 512], BF16, "gT")
    l_tok = P.sb([128, 4, 256], F32, "l_tok")
    ebT = P.sb([128, 2, 512], BF16, "ebT")
    enbT = P.sb([128, 2, 512], BF16, "enbT")
    eblast = P.sb([128, 2, 4], F32, "eblast")
    ek = Rot([P.sb([128, 256], F32, f"ek{i}") for i in range(2)])
    khat = P.sb([128, 4, 256], BF16, "khat")
    base_vg = P.sb([128, 1024], F32, "base_vg")
    vg = Tile(base_vg[:, :].bitcast(BF16).rearrange("p (j n) -> p j n", j=4), "vg", toks=[Tok("vg0"), Tok("vg1")])
    base_sr = P.sb([128, 1024], F32, "base_sr")
    sr = Tile(base_sr[:, :].bitcast(BF16).rearrange("p (j n) -> p j n", j=4), "sr", toks=[Tok("sr0"), Tok("sr1")])
    state_f = P.sb([128, 2, 128], F32, "state_f")
    state_b = P.sb([128, 2, 128], BF16, "state_b")
    base_oa = P.sb([128, 1024], F32, "base_oa")
    oa_b = base_oa[:, :].bitcast(BF16)
    oa_tok = [Tile(oa_b[:, i * 512:(i + 1) * 512], f"oa_tok{i}") for i in range(4)]
    ob_tiles = [P.sb([128, 512], BF16, f"ob_tok{i}") for i in range(2)]
    ob_tok = Rot(ob_tiles)
    base_oT = P.sb([128, 2048], F32, "base_oT")
    oT = Tile(base_oT[:, :].bitcast(BF16).rearrange("p (n w t) -> p n w t", n=2, w=4), "oT")
    oT_tok = [[Tok(f"oT{n}_{j}") for j in range(4)] for n in range(2)]
    base_PT = P.sb([128, 1024], F32, "base_PT")
    PT_b = base_PT[:, :].bitcast(BF16)
    PT_tiles = [Tile(PT_b[:, i * 512:(i + 1) * 512], f"PT{i}") for i in range(4)]
    PT = Rot(PT_tiles)
    AT = Rot([P.sb([128, 128], BF16, f"AT{i}") for i in range(2)])
    qn = Rot([Tile(base_PT[:, i * 512:(i + 1) * 512], f"qn{i}", toks=PT_tiles[2 * i].toks + PT_tiles[2 * i + 1].toks)
              for i in range(2)])
    qr = Rot([Tile(base_oa[:, i * 512:(i + 1) * 512], f"qr{i}", toks=oa_tok[2 * i].toks + oa_tok[2 * i + 1].toks)
              for i in range(2)])
    qb = Rot(ob_tiles)
    hb = Rot([P.sb([128, 1024], BF16, f"hb{i}") for i in range(1)])
    m0 = Rot([Tile(base_sr[:, i * 512:(i + 1) * 512], f"m0{i}", toks=[sr.toks[i]]) for i in range(2)]
             + [Tile(base_vg[:, i * 512:(i + 1) * 512], f"m0v{i}", toks=[vg.toks[i]]) for i in range(2)])
    o_sb = Rot([P.sb([128, 128], F32, f"o_sb{i}") for i in range(3)])
    oT_b = base_oT[:, :].bitcast(BF16)
    h2b = Rot([Tile(oT_b[:, 2048 + i * 1024:2048 + (i + 1) * 1024], f"h2b{i}", toks=oT_tok[1]) for i in range(2)])
    h2T = Tile(base_oT[:, 0:1024].rearrange("p (c t) -> p c t", c=8), "h2T", toks=oT_tok[0])
    st = Rot([P.sb([128, 8], F32, f"st{i}") for i in range(16)])
    s36 = Rot([P.sb([128, 36], F32, f"s36{i}") for i in range(12)])
    slots_i = P.sb([128, NT, 2], I32, "slots_i")
    slots_f = P.sb([128, NT, 2], F32, "slots_f")
    cw_all = P.sb([128, NT, 2], F32, "cw_all")

    banks = [P.ps([128, 512], F32, f"bank{i}") for i in range(8)]
    gen_banks = Rot(banks[0:4])
    acc_tile = {}
    acc_tok = {}
    for m in range(2):
        for jq in range(4):
            acc_tok[(m, jq)] = banks[4 + jq].toks[0]
            acc_tile[(m, jq)] = banks[4 + jq][:, m * 129:(m + 1) * 129]

    def fsz(ap):
        n_ = 1
        for d_ in ap.shape[1:]:
            n_ *= d_
        return n_

    def MM(out, lhsT, rhs, start=True, stop=True, R=(), W=(), sgc=False):
        c_ = (0.08 + fsz(out) / 2700.0) * (3.0 if lhsT.dtype == F32 else 1.0) * (2.0 if lhsT.shape[0] == 64 else 1.0)
        if sgc:
            P.op('pe', lambda e: e.matmul(out, lhsT=lhsT, rhs=rhs, start=start, stop=stop, skip_group_check=True), R, W, cost=c_)
        else:
            P.op('pe', lambda e: e.matmul(out, lhsT=lhsT, rhs=rhs, start=start, stop=stop), R, W, cost=c_)

    def TR(out, in_, ident, R=(), W=()):
        c_ = (0.08 + fsz(out) / 2700.0) * (3.0 if in_.dtype == F32 else 1.0)
        P.op('pe', lambda e: e.transpose(out, in_, ident), R, W, cost=c_)

    def ACT(out, in_, func, R=(), W=(), bias=None, scale=None, accum=None):
        kw = {}
        if bias is not None:
            kw['bias'] = bias
        if scale is not None:
            kw['scale'] = scale
        if accum is not None:
            kw['accum_out'] = accum
        P.op('act', lambda e: e.activation(out=out, in_=in_, func=func, **kw), R, W, cost=(fsz(out) + 190) / 1200.0)

    def TT(out, in0, in1, op, R=(), W=(), eng='dve'):
        P.op(eng, lambda e: e.tensor_tensor(out=out, in0=in0, in1=in1, op=op), R, W, cost=(fsz(out) + 120) / 960.0)

    def TS(out, in0, s1, op0, R=(), W=(), s2=None, op1=None, eng='dve'):
        if op1 is None:
            P.op(eng, lambda e: e.tensor_scalar(out=out, in0=in0, scalar1=s1, scalar2=None, op0=op0), R, W,
                 cost=(fsz(out) * 0.6 + 120) / 960.0)
        else:
            P.op(eng, lambda e: e.tensor_scalar(out=out, in0=in0, scalar1=s1, scalar2=s2, op0=op0, op1=op1), R, W,
                 cost=(fsz(out) * 0.6 + 120) / 960.0)

    def STT(out, in0, scalar, in1, op0, op1, R=(), W=(), accum=None, eng='dve'):
        if accum is None:
            P.op(eng, lambda e: e.scalar_tensor_tensor(out=out, in0=in0, scalar=scalar, in1=in1, op0=op0, op1=op1), R, W,
                 cost=(fsz(out) + 120) / 960.0)
        else:
            P.op(eng, lambda e: e.scalar_tensor_tensor(out=out, in0=in0, scalar=scalar, in1=in1, op0=op0, op1=op1,
                                                       accum_out=accum), R, W, cost=(fsz(out) + 120) / 960.0)

    def CP(out, in_, R=(), W=(), eng='dve'):
        if eng == 'act':
            P.op('act', lambda e: e.copy(out=out, in_=in_), R, W, cost=(fsz(out) + 300) / 1200.0)
        else:
            P.op(eng, lambda e: e.tensor_copy(out=out, in_=in_), R, W, cost=(fsz(out) * 0.6 + 120) / 960.0)

    def RED(out, in_, op, R=(), W=()):
        P.op('dve', lambda e: e.tensor_reduce(out=out, in_=in_, axis=AX.X, op=op), R, W, cost=(fsz(in_) + 120) / 960.0)

    def RECIP(out, in_, R=(), W=()):
        P.op('dve', lambda e: e.reciprocal(out=out, in_=in_), R, W, cost=(fsz(out) * 5.5 + 150) / 960.0)

    def MEMSET(ap, val, W=(), eng='dve'):
        P.op(eng, lambda e: e.memset(ap, val), (), W, cost=(fsz(ap) * 0.5 + 100) / 960.0)

    def DMA(q, out, in_, R=(), W=()):
        nb_ = out.shape[0] * fsz(out) * (4 if in_.dtype == F32 else 2)
        P.dma(q, lambda e: e.dma_start(out=out, in_=in_), R, W, nbytes=nb_)

    def rms_scale(ssq, n, tp):
        pass

    wAc_d = P.dram("wAc", [15, 128, 4096], BF16)
    wAc_tok = [Tok(f"wAc{i}") for i in range(15)]
    wsrc = []
    for g in [-1] + list(range(NG)):
        if g < 0:
            wsrc += [(g, i) for i in (0, 2, 3, 5)]
        else:
            wsrc += [(g, i) for i in (0, 1, 2, 3, 4, 5, 6, 11, 7, 8, 12, 9, 10, 13, 14)]
    wstate = dict(load=0, use=0)

    def w_issue():
        k = wstate['load']
        if k < len(wsrc):
            buf = wbufs[k % NWB]
            g, i = wsrc[k]
            if g <= 0:
                DMA('pool', buf[:, :], wA_d[i], R=[], W=[buf])
                if g == 0 and NG > 1:
                    DMA('sp', wAc_d[i], buf[:, :], R=[buf], W=[wAc_tok[i]])
            else:
                DMA('sp', buf[:, :], wAc_d[i], R=[wAc_tok[i]], W=[buf])
            wstate['load'] += 1

    def w_get(off=0):
        return wbufs[(wstate['use'] + off) % NWB]

    def w_done(n=1):
        for _ in range(n):
            wstate['use'] += 1
            w_issue()

    DMA('sp', vecs[:, :], vec_d[0:1, :].partition_broadcast(128), W=[vecs])
    DMA('sp', cm[:, :], cm_d[:, :], W=[cm])
    DMA('sp', gmixT[:, :], gmixT_d[:, :], W=[gmixT])
    DMA('sp', bmgT[:, :], bmgT_d[:, :], W=[bmgT])
    DMA('sp', wr_sb[:, :], wr_d[:, :], W=[wr_sb])
    DMA('sp', wup_f[:, :], wup_d[:, :], W=[wup_f])
    DMA('sp', cntb[:, :], ebase_d[0:1, :].partition_broadcast(128), W=[cntb])
    DMA('sp', limb[:, :], ebase_d[0:1, :].partition_broadcast(128), W=[limb])
    TS(limb[:, :], limb[:, :], float(CAP), ALU.add, R=[limb], W=[limb])
    MEMSET(trashc[:, :], float(NSLOT), W=[trashc])
    for _ in range(NWB):
        w_issue()
    MEMSET(base_oT[:, :], 0.0, W=oT_tok[0] + oT_tok[1])
    CP(cmb[:, :], cm[:, 0:512], R=[cm], W=[cmb])
    TS(nbmgT[:, :], bmgT[:, :], -1.0, ALU.mult, R=[bmgT], W=[nbmgT])
    CP(wup_b[:, :], wup_f[:, :], R=[wup_f], W=[wup_b])
    MEMSET(ones_f[:, :], 1.0, W=[ones_f])
    MEMSET(state_f[:, :, :], 0.0, W=[state_f])
    MEMSET(state_b[:, :, :], 0.0, W=[state_b])
    MEMSET(arena[:, AR_KT:AR_KT + AR_VA], 1.0, W=[VA[g] for g in VA])
    for i in range(2):
        STT(junk_d[:, 0:64], vecs[:, V_LAM + 128 * i:V_LAM + 128 * i + 64], 1.0,
            vecs[:, V_LAM + 128 * i + 64:V_LAM + 128 * i + 128], ALU.mult, ALU.mult,
            R=[vecs], W=[junk_d, lamt], accum=lamt[:, i:i + 1])
    ACT(lamt[:, 2:4], lamt[:, 0:2], AF.Exp, R=[lamt], W=[lamt])
    TT(lamt[:, 0:1], lamt[:, 3:4], lamt[:, 2:3], ALU.subtract, R=[lamt], W=[lamt])
    TS(neglam[:, :], lamt[:, 0:1], -LAM_INIT, ALU.add, R=[lamt], W=[neglam])
    TS(gsub_s[:, :], vecs[:, V_GSUB:V_GSUB + 128], 1.0 - LAM_INIT, ALU.mult, R=[vecs], W=[gsub_s])

    def stat():
        return st()

    def rstd_from_ssq(ss_tile, ncol, n, tp):
        ACT(ss_tile[:tp, 0:ncol], ss_tile[:tp, 0:ncol], AF.Ln, R=[ss_tile], W=[ss_tile], bias=EPS, scale=1.0 / n)
        ACT(ss_tile[:tp, 0:ncol], ss_tile[:tp, 0:ncol], AF.Exp, R=[ss_tile], W=[ss_tile], scale=-0.5)

    def qk_norm_rope(ps_bank, tp, j, gain_off, dst_T, dst_cols):
        s8 = stat()
        ACT(junk_f[:tp, 0:512], ps_bank[:tp, :], AF.Square, R=[ps_bank], W=[junk_f])
        RED(s8[:tp, 0:8], junk_f[:tp, 0:512].rearrange("p (g d) -> p g d", d=64), ALU.add, R=[junk_f], W=[s8])
        rstd_from_ssq(s8, 8, 64, tp)
        a = qn()
        TT(a[:tp, :].rearrange("p (g d) -> p g d", d=64), ps_bank[:tp, :].rearrange("p (g d) -> p g d", d=64),
           s8[:tp, 0:8].unsqueeze(2).to_broadcast([tp, 8, 64]), ALU.mult, R=[ps_bank, s8], W=[a])
        TT(a[:tp, :].rearrange("p (g d) -> p g d", d=64), a[:tp, :].rearrange("p (g d) -> p g d", d=64),
           vecs[:tp, gain_off:gain_off + 64].unsqueeze(1).to_broadcast([tp, 8, 64]), ALU.mult, R=[a, vecs], W=[a])
        r = qr()
        a16 = a[:tp, :].rearrange("p (g d) -> p g d", d=32)
        a4 = a[:tp, :].rearrange("p (g two d) -> p g two d", two=2, d=32)
        r4 = r[:tp, :].rearrange("p (g two d) -> p g two d", two=2, d=32)
        cosb = ropet[:tp, j, 0:32].unsqueeze(1).to_broadcast([tp, 8, 32])
        sinb = ropet[:tp, j, 32:64].unsqueeze(1).to_broadcast([tp, 8, 32])
        nsinb = ropet[:tp, j, 64:96].unsqueeze(1).to_broadcast([tp, 8, 32])
        TT(r4[:, :, 0, :], a4[:, :, 1, :], nsinb, ALU.mult, R=[a, ropet], W=[r])
        TT(r4[:, :, 1, :], a4[:, :, 0, :], sinb, ALU.mult, R=[a, ropet], W=[r])
        TT(a16, a16, ropet[:tp, j, 0:32].unsqueeze(1).to_broadcast([tp, 16, 32]), ALU.mult, R=[a, ropet], W=[a])
        b_ = qb()
        TT(b_[:tp, :], a[:tp, :], r[:tp, :], ALU.add, R=[a, r], W=[b_])
        bk = gen_banks()
        bkb = bk[:, :].bitcast(BF16)
        for h in range(4):
            TR(bkb[:, h * 128:h * 128 + tp], b_[:tp, h * 128:(h + 1) * 128], ident_b[:tp, :tp], R=[b_, cmb], W=[bk])
        src_ = bkb[:, 0:512].rearrange("p (h t) -> p h t", h=4)
        if dst_T is QT:
            CP(QT[0:64, :, 0, dst_cols], src_[0:64, :, 0:tp], R=[bk], W=[dst_T], eng='act')
            CP(QT[64:128, :, 1, dst_cols], src_[64:128, :, 0:tp], R=[bk], W=[dst_T], eng='dve')
        else:
            CP(dst_T[:, :, dst_cols], src_[:, :, 0:tp], R=[bk], W=[dst_T], eng='act')

    def mark(name):
        P.marks.append((name, len(P.ops)))

    def phaseA(gi):
        meta = gi < 0
        nt = 1 if meta else 4
        tp = 16 if meta else 128
        ntok = nt * tp
        pos0 = 0 if meta else 16 + gi * 512
        tok0 = 0 if meta else gi * 512
        if meta:
            DMA('sp', ropet[:16, 0, :], rope_d[0:16, :], W=[ropet])
        else:
            DMA('sp', ropet[:, :, :], rope_d[pos0:pos0 + 512, :].rearrange("(j p) c -> p j c", p=128), W=[ropet])
        for j in range(nt):
            xt = frow()
            if meta:
                DMA('sp', xt[:16, :], meta_d[:, :], W=[xt])
            else:
                DMA('sp', xt[:, :], x_d[tok0 + j * 128:tok0 + (j + 1) * 128, :], W=[xt])
            s = stat()
            ACT(junk_b[:tp, :], xt[:tp, :], AF.Square, R=[xt], W=[junk_b, s], accum=s[:tp, 0:1])
            rstd_from_ssq(s, 1, 1024, tp)
            h_ = hb()
            TS(h_[:tp, :], xt[:tp, :], s[:tp, 0:1], ALU.mult, R=[xt, s], W=[h_])
            bk = gen_banks()
            bkb = bk[:, :].bitcast(BF16)
            for c in range(8):
                TR(bkb[:, c * 128:c * 128 + tp], h_[:tp, c * 128:(c + 1) * 128], ident_b[:tp, :tp], R=[h_, cmb], W=[bk])
            TT(hT[:, :, j * tp:(j + 1) * tp], bkb[:, :].rearrange("p (c t) -> p c t", c=8)[:, :, 0:tp],
               gmixT[:, :].unsqueeze(2).to_broadcast([128, 8, tp]), ALU.mult, R=[bk, gmixT], W=[hT])

        def tm_block(wt, j, ncols, col0=0):
            w3 = wt[:, :].rearrange("p (c n) -> p c n", c=8)
            bk = gen_banks()
            for c in range(8):
                MM(bk[:tp, 0:ncols], lhsT=hT[:, c, j * tp:(j + 1) * tp], rhs=w3[:, c, col0:col0 + ncols],
                   start=(c == 0), stop=(c == 7), R=[hT, wt], W=[bk])
            return bk

        def fm_chunk(wt, col0, ncol):
            w3 = wt[:, :].rearrange("p (c n) -> p c n", c=8)
            bk = gen_banks()
            for c in range(8):
                MM(bk[:ncol, 0:ntok], lhsT=w3[:, c, col0:col0 + ncol], rhs=hT[:, c, 0:ntok],
                   start=(c == 0), stop=(c == 7), R=[hT, wt], W=[bk])
            return bk

        w0 = w_get()
        bk = fm_chunk(w0, 256, 16)
        CP(gT[:, 0:ntok], bk[:16, 0:ntok], R=[bk], W=[gT], eng='act')
        for j in range(nt):
            bk = gen_banks()
            MM(bk[:tp, 0:256], lhsT=gT[:, j * tp:(j + 1) * tp], rhs=wup_b[:, :], R=[gT, wup_b], W=[bk])
            TT(l_tok[:tp, j, :], bk[:tp, 0:256], vecs[:tp, V_BGATE:V_BGATE + 256], ALU.add, R=[bk, vecs], W=[l_tok])
        ACT(l_tok[:tp, 0:nt, :], l_tok[:tp, 0:nt, :], AF.Exp, R=[l_tok], W=[l_tok], scale=-1.0)
        ACT(l_tok[:tp, 0:nt, :], l_tok[:tp, 0:nt, :], AF.Ln, R=[l_tok], W=[l_tok], bias=1.0)
        for j in range(nt):
            bk = gen_banks()
            MM(bk[:tp, 0:256], lhsT=sgtS_f[:tp, :tp], rhs=l_tok[:tp, j, :], R=[cm, l_tok], W=[bk])
            e_ = ek()
            ACT(e_[:tp, :], bk[:tp, 0:256], AF.Exp, R=[bk], W=[e_])
            bk2 = tm_block(w0, j, 256, 0)
            TT(khat[:tp, j, :], bk2[:tp, 0:256], e_[:tp, :], ALU.mult, R=[bk2, e_], W=[khat])
            if not meta:
                bk3 = gen_banks()
                for p in range(2):
                    MM(bk3[:, p * 128:p * 128 + tp], lhsT=l_tok[:tp, j, p * 128:(p + 1) * 128], rhs=triS_f[:tp, :tp],
                       R=[l_tok, cm], W=[bk3])
                v3 = bk3[:, 0:256].rearrange("p (a t) -> p a t", a=2)
                ACT(ebT[:, :, j * 128:(j + 1) * 128], v3, AF.Exp, R=[bk3], W=[ebT])
                ACT(enbT[:, :, j * 128:(j + 1) * 128], v3, AF.Exp, R=[bk3], W=[enbT], scale=-1.0)
                ACT(eblast[:, :, j:j + 1], v3[:, :, 127:128], AF.Exp, R=[bk3], W=[eblast])
        w_done()
        if not meta:
            w1 = w_get()
            for p in range(2):
                bk = fm_chunk(w1, p * 128, 128)
                STT(qtT[:, p, :], bk[:, 0:512], 0.125, ebT[:, p, :], ALU.mult, ALU.mult, R=[bk, ebT], W=[qtT])
            for p in range(2):
                bk = fm_chunk(w1, 256 + p * 128, 128)
                TT(ktT[:, p, :], bk[:, 0:512], enbT[:, p, :], ALU.mult, R=[bk, enbT], W=[ktT])
            w_done()
        wt = w_get()
        for j in range(nt):
            bk = tm_block(wt, j, 512)
            qk_norm_rope(bk, tp, j, V_GK, KT[gi], slice(j * tp, (j + 1) * tp))
        w_done()
        wt = w_get()
        for j in range(nt):
            bk = tm_block(wt, j, 512)
            CP(VA[gi][:tp, j, :, 0:128], bk[:tp, :].rearrange("p (h e) -> p h e", h=4), R=[bk], W=[VA[gi]], eng='act')
        w_done()
        if not meta:
            MEMSET(scr8[:, :], 0.0, W=[QT])
            wt = w_get()
            for j in range(nt):
                bk = tm_block(wt, j, 512)
                qk_norm_rope(bk, tp, j, V_GQ, QT, slice(j * 128, (j + 1) * 128))
            w_done()
        wt = w_get()
        for j in range(nt):
            bk = tm_block(wt, j, 512)
            CP(vg[:tp, j, :], bk[:tp, :], R=[bk], W=[vg], eng='act')
        w_done()
        if not meta:
            wt = w_get()
            for j in range(nt):
                bk = tm_block(wt, j, 512)
                ACT(sr[:, j, :], bk[:, :], AF.Silu, R=[bk], W=[sr])
            w_done()

        mark(f'g{gi}.inproj')
        if gi == 0:
            zsrc = base_oT[:, :].bitcast(BF16)
            DMA('sp', Yg_d[NSLOT:NSLOT + 128, :], base_oT[:, 0:1024], R=oT_tok[0] + oT_tok[1], W=[Yg_d])
            for zb in range(NSLOT // 512):
                DMA('sp', Xg_d[zb * 512:(zb + 1) * 512, :].rearrange("(p r) d -> p (r d)", r=4), zsrc,
                    R=oT_tok[0] + oT_tok[1], W=[Xg_d])
        if not meta:
            ktiles = [(-1, 0, 16)]
            for g2 in range(gi):
                for j2 in range(4):
                    ktiles.append((g2, j2, 128))
            items = []
            for h in range(4):
                for (g2, j2, nk) in ktiles + [(gi, r, 128) for r in range(4)]:
                    for m in range(2):
                        items.append(dict(h=h, g2=g2, j2=j2, nk=nk, m=m))

            def emit_S(it):
                h, g2, j2, nk, m = it['h'], it['g2'], it['j2'], it['nk'], it['m']
                diag = (g2 == gi)
                jq0 = j2 if diag else 0
                nq = 512 - jq0 * 128
                kT = KT[g2][:, h, j2 * nk:(j2 + 1) * nk] if g2 >= 0 else KT[-1][:, h, 0:16]
                bk = gen_banks()
                if diag:
                    MM(bk[:, 0:128], lhsT=ident_b, rhs=maskb_b, start=True, stop=True, R=[cmb], W=[bk])
                    MM(bk[:, 0:nq], lhsT=kT, rhs=QT[:, h, m, jq0 * 128:512], start=False, stop=True, sgc=True,
                       R=[KT[g2], QT], W=[bk])
                else:
                    MM(bk[:nk, 0:512], lhsT=kT, rhs=QT[:, h, m, 0:512], R=[KT[g2], QT], W=[bk])
                pt = PT()
                ACT(pt[:nk, 0:nq], bk[:nk, 0:nq], AF.Exp, R=[bk], W=[pt], scale=0.125)
                it['pt'] = pt
                it['jq0'] = jq0

            def emit_PV(it):
                h, g2, j2, nk, m = it['h'], it['g2'], it['j2'], it['nk'], it['m']
                diag = (g2 == gi)
                jq0 = it['jq0']
                pt = it['pt']
                if g2 == -1 and m == 0:
                    for bi in range(4):
                        MEMSET(banks[4 + bi][:, 0:258], 0.0, W=[banks[4 + bi]])
                for jq in range(jq0, 4):
                    MM(acc_tile[(m, jq)], lhsT=pt[:nk, (jq - jq0) * 128:(jq - jq0 + 1) * 128],
                       rhs=VA[g2][:nk, j2, h, :], start=False, stop=False, sgc=True,
                       R=[pt, VA[g2]], W=[acc_tok[(m, jq)]])
                if diag and m == 1:
                    jq = j2
                    a0, a1 = acc_tile[(0, jq)], acc_tile[(1, jq)]
                    t0 = acc_tok[(0, jq)]
                    s = stat()
                    RECIP(s[:, 0:1], a0[:, 128:129], R=[t0], W=[s])
                    RECIP(s[:, 1:2], a1[:, 128:129], R=[t0], W=[s])
                    TT(s[:, 2:3], s[:, 1:2], neglam[:, :], ALU.mult, R=[s, neglam], W=[s])
                    o1 = o_sb()
                    TS(o1[:, :], a1[:, 0:128], s[:, 2:3], ALU.mult, R=[t0, s], W=[o1])
                    o = o_sb()
                    s2 = stat()
                    STT(o[:, :], a0[:, 0:128], s[:, 0:1], o1[:, :], ALU.mult, ALU.add, R=[t0, s, o1], W=[o])
                    STT(junk_d2[:, :], o[:, :], 1.0, o[:, :], ALU.mult, ALU.mult, R=[o], W=[junk_d2, s2], accum=s2[:, 0:1])
                    rstd_from_ssq(s2, 1, 128, 128)
                    STT(oa_tok[jq][:, h * 128:(h + 1) * 128], o[:, :], s2[:, 0:1], gsub_s[:, :], ALU.mult, ALU.mult,
                        R=[o, s2, gsub_s], W=[oa_tok[jq]])

            LA = 2
            for i in range(len(items) + LA):
                if i < len(items):
                    emit_S(items[i])
                if i >= LA:
                    emit_PV(items[i - LA])
            for jq in range(4):
                if dbg:
                    f = frow()
                    CP(f[:, 0:512], oa_tok[jq][:, :], R=[oa_tok[jq]], W=[f])
                    DMA('sp', dbg_d["d_oa"][tok0 + jq * 128:tok0 + (jq + 1) * 128, :], f[:, 0:512], R=[f], W=[dbg_d["d_oa"]])
                bk = gen_banks()
                bkb = bk[:, :].bitcast(BF16)
                for h in range(4):
                    TR(bkb[:, h * 128:(h + 1) * 128], oa_tok[jq][:, h * 128:(h + 1) * 128], ident_b, R=[oa_tok[jq], cmb], W=[bk])
                CP(oT[:, 0, :, jq * 128:(jq + 1) * 128], bkb[:, 0:512].rearrange("p (h t) -> p h t", h=4),
                   R=[bk], W=[oT_tok[0][jq]], eng='act')

        mark(f'g{gi}.attn')
        for j in range(nt):
            cols = slice(j * tp, (j + 1) * tp)
            ob = ob_tok() if not meta else None
            for h in range(4):
                p, hh = h // 2, h % 2
                rows = slice(hh * 64, (hh + 1) * 64)
                if not meta:
                    bk = gen_banks()
                    MM(bk[:, 0:128], lhsT=ktT[rows, p, cols], rhs=qtT[rows, p, cols], R=[ktT, qtT], W=[bk])
                    at = AT()
                    TT(at[:, :], bk[:, 0:128], tri01_f, ALU.mult, R=[bk, cm], W=[at])
                    bo = gen_banks()
                    MM(bo[:, 0:128], lhsT=at[:, :], rhs=vg[:, j, h * 128:(h + 1) * 128], start=True, stop=False, R=[at, vg], W=[bo])
                    MM(bo[:, 0:128], lhsT=qtT[rows, p, cols], rhs=state_b[rows, p, :], start=False, stop=True,
                       R=[qtT, state_b], W=[bo])
                bkv = gen_banks()
                MM(bkv[:, 0:128], lhsT=khat[:tp, j, p * 128:(p + 1) * 128], rhs=vg[:tp, j, h * 128:(h + 1) * 128],
                   R=[khat, vg], W=[bkv])
                if meta:
                    CP(state_f[rows, p, :], bkv[rows, 0:128], R=[bkv], W=[state_f])
                else:
                    STT(state_f[rows, p, :], state_f[rows, p, :], eblast[rows, p, j:j + 1], bkv[rows, 0:128], ALU.mult, ALU.add,
                        R=[state_f, eblast, bkv], W=[state_f])
                CP(state_b[rows, p, :], state_f[rows, p, :], R=[state_f], W=[state_b], eng='act')
                if not meta:
                    s2 = stat()
                    ACT(junk_f[:, 0:128], bo[:, 0:128], AF.Square, R=[bo], W=[junk_f, s2], accum=s2[:, 0:1])
                    rstd_from_ssq(s2, 1, 128, 128)
                    o = o_sb()
                    STT(o[:, :], bo[:, 0:128], s2[:, 0:1], vecs[:, V_GGLA:V_GGLA + 128], ALU.mult, ALU.mult, R=[bo, s2, vecs], W=[o])
                    TT(ob[:, h * 128:(h + 1) * 128], o[:, :], sr[:, j, h * 128:(h + 1) * 128], ALU.mult, R=[o, sr], W=[ob])
            if not meta:
                if dbg:
                    f = frow()
                    CP(f[:, 0:512], ob[:, :], R=[ob], W=[f])
                    DMA('sp', dbg_d["d_ob"][tok0 + j * 128:tok0 + (j + 1) * 128, :], f[:, 0:512], R=[f], W=[dbg_d["d_ob"]])
                bk = gen_banks()
                bkb = bk[:, :].bitcast(BF16)
                for h in range(4):
                    TR(bkb[:, h * 128:(h + 1) * 128], ob[:, h * 128:(h + 1) * 128], ident_b, R=[ob, cmb], W=[bk])
                CP(oT[:, 1, :, j * 128:(j + 1) * 128], bkb[:, 0:512].rearrange("p (h t) -> p h t", h=4),
                   R=[bk], W=[oT_tok[1][j]], eng='act')
        if meta:
            return

        mark(f'g{gi}.gla')
        for half2 in range(2):
            BR, G_a, G_b = w_get(0), w_get(1), w_get(2)
            BR5 = BR[:, :].rearrange("p (gbl n wc col) -> p gbl n wc col", gbl=2, n=2, wc=4)
            for gbl, G in ((0, G_a), (1, G_b)):
                gb = half2 * 2 + gbl
                G3 = G[:, :].rearrange("p (c n) -> p c n", c=8)
                for cl in range(2):
                    c = 2 * gb + cl
                    parts = []
                    for n_ in range(2):
                        bg = gen_banks()
                        for cc in range(8):
                            MM(bg[:, :], lhsT=G3[:, cc, n_ * 256 + cl * 128:n_ * 256 + (cl + 1) * 128], rhs=hT[:, cc, :],
                               start=(cc == 0), stop=(cc == 7), R=[G, hT], W=[bg])
                        s_ = m0()
                        ACT(s_[:, :], bg[:, :], AF.Sigmoid, R=[bg, bmgT], W=[s_], bias=bmgT[:, n_ * 8 + c:n_ * 8 + c + 1])
                        by = gen_banks()
                        for wc in range(4):
                            MM(by[:, :], lhsT=BR5[:, gbl, n_, wc, cl * 128:(cl + 1) * 128], rhs=oT[:, n_, wc, :],
                               start=(wc == 0), stop=(wc == 3), R=[BR] + oT_tok[n_], W=[by])
                        TT(s_[:, :], by[:, :], s_[:, :], ALU.mult, R=[by, s_], W=[s_])
                        parts.append(s_)
                    m_, m2 = parts
                    TT(mT[:, c, :], m_[:, :], m2[:, :], ALU.add, R=[m_, m2], W=[mT], eng='pool')
            w_done(3)

        mark(f'g{gi}.merge')
        O0, O1 = w_get(0), w_get(1)
        O3 = [O0[:, :].rearrange("p (c n) -> p c n", c=8), O1[:, :].rearrange("p (c n) -> p c n", c=8)]
        Ot = [O0, O1]
        for j in range(4):
            gj = gi * 4 + j
            rows_d = slice(tok0 + j * 128, tok0 + (j + 1) * 128)
            xt = frow()
            DMA('sp', xt[:, :], x_d[rows_d, :], W=[xt])
            u = frow()
            for half in range(2):
                bk = gen_banks()
                for c in range(8):
                    MM(bk[:, :], lhsT=mT[:, c, j * 128:(j + 1) * 128], rhs=O3[half][:, c, :], start=(c == 0), stop=(c == 7),
                       R=[mT, Ot[half]], W=[bk])
                TT(u[:, half * 512:(half + 1) * 512], bk[:, :], xt[:, half * 512:(half + 1) * 512], ALU.add, R=[bk, xt], W=[u])
            DMA('sp', U_d[rows_d, :], u[:, :], R=[u], W=[U_d])
            if dbg:
                DMA('sp', dbg_d["d_u"][rows_d, :], u[:, :], R=[u], W=[dbg_d["d_u"]])
            s = stat()
            ACT(junk_b[:, :], u[:, :], AF.Square, R=[u], W=[junk_b, s], accum=s[:, 0:1])
            rstd_from_ssq(s, 1, 1024, 128)
            h2f = frow()
            STT(h2f[:, :], u[:, :], s[:, 0:1], vecs[:, V_GFFN:V_GFFN + 1024], ALU.mult, ALU.mult, R=[u, s, vecs], W=[h2f])
            h2 = h2b()
            CP(h2[:, :], h2f[:, :], R=[h2f], W=[h2], eng='act')
            for q4 in range(2):
                bk = gen_banks()
                for c4 in range(4):
                    c = q4 * 4 + c4
                    TR(bk[:, c4 * 128:(c4 + 1) * 128], h2f[:, c * 128:(c + 1) * 128], ident_f, R=[h2f, cm], W=[bk])
                CP(h2T[:, q4 * 4:(q4 + 1) * 4, :], bk[:, :].rearrange("p (c t) -> p c t", c=4), R=[bk], W=[h2T],
                   eng=('act' if q4 == 0 else 'dve'))
            bl = gen_banks()
            wr3 = wr_sb[:, :].rearrange("p (c n) -> p c n", c=8)
            for c in range(8):
                MM(bl[:, 0:36], lhsT=h2T[:, c, :], rhs=wr3[:, c, :], start=(c == 0), stop=(c == 7), R=[h2T, wr_sb], W=[bl])
            lg = s36()
            TT(lg[:, :], bl[:, 0:36], vecs[:, V_BR:V_BR + 36], ALU.add, R=[bl, vecs], W=[lg])
            if dbg:
                DMA('sp', dbg_d["d_lg"][rows_d, :], lg[:, :], R=[lg], W=[dbg_d["d_lg"]])
            s = stat()
            RED(s[:, 0:1], lg[:, 0:4], ALU.max, R=[lg], W=[s])
            gone = stat()
            TS(gone[:, 0:4], lg[:, 0:4], s[:, 0:1], ALU.is_equal, R=[lg, s], W=[gone])
            TS(s[:, 1:2], s[:, 0:1], -1.0, ALU.mult, R=[s], W=[s])
            ACT(junk_f[:, 0:4], lg[:, 0:4], AF.Exp, R=[lg, s], W=[junk_f, s], bias=s[:, 1:2], accum=s[:, 2:3])
            RECIP(s[:, 3:4], s[:, 2:3], R=[s], W=[s])
            t36 = s36()
            TT(t36[:, 0:32].rearrange("p (g e) -> p g e", g=4), lg[:, 4:36].rearrange("p (g e) -> p g e", g=4),
               gone[:, 0:4].unsqueeze(2).to_broadcast([128, 4, 8]), ALU.mult, R=[lg, gone], W=[t36])
            es_ = stat()
            RED(es_[:, 0:8], t36[:, 0:32].rearrange("p (g e) -> p e g", g=4), ALU.add, R=[t36], W=[es_])
            RED(s[:, 4:5], es_[:, 0:8], ALU.max, R=[es_], W=[s])
            one1 = stat()
            TS(one1[:, 0:8], es_[:, 0:8], s[:, 4:5], ALU.is_equal, R=[es_, s], W=[one1])
            es2 = stat()
            STT(es2[:, 0:8], one1[:, 0:8], -1e30, es_[:, 0:8], ALU.mult, ALU.add, R=[one1, es_], W=[es2])
            RED(s[:, 5:6], es2[:, 0:8], ALU.max, R=[es2], W=[s])
            one2 = stat()
            TS(one2[:, 0:8], es2[:, 0:8], s[:, 5:6], ALU.is_equal, R=[es2, s], W=[one2])
            TT(s[:, 6:7], s[:, 5:6], s[:, 4:5], ALU.subtract, R=[s], W=[s])
            ACT(s[:, 7:8], s[:, 6:7], AF.Exp, R=[s], W=[s])
            w_ = stat()
            TS(w_[:, 0:1], s[:, 7:8], 1.0, ALU.add, R=[s], W=[w_])
            RECIP(w_[:, 1:2], w_[:, 0:1], R=[w_], W=[w_])
            TT(w_[:, 2:3], s[:, 7:8], w_[:, 1:2], ALU.mult, R=[s, w_], W=[w_])
            TS(cw_all[:, gj, :], w_[:, 1:3], s[:, 3:4], ALU.mult, R=[w_, s], W=[cw_all])
            M1 = s36()
            TT(M1[:, 0:32].rearrange("p (g e) -> p g e", g=4), gone[:, 0:4].unsqueeze(2).to_broadcast([128, 4, 8]),
               one1[:, 0:8].unsqueeze(1).to_broadcast([128, 4, 8]), ALU.mult, R=[gone, one1], W=[M1])
            M2 = s36()
            TT(M2[:, 0:32].rearrange("p (g e) -> p g e", g=4), gone[:, 0:4].unsqueeze(2).to_broadcast([128, 4, 8]),
               one2[:, 0:8].unsqueeze(1).to_broadcast([128, 4, 8]), ALU.mult, R=[gone, one2], W=[M2])
            Mt = s36()
            TT(Mt[:, 0:32], M1[:, 0:32], M2[:, 0:32], ALU.add, R=[M1, M2], W=[Mt])
            bp = gen_banks()
            MM(bp[:, 0:32], lhsT=slt01_f, rhs=Mt[:, 0:32], R=[cm, Mt], W=[bp])
            MM(bp[:, 32:64], lhsT=ones_f[:, :], rhs=Mt[:, 0:32], R=[ones_f, Mt], W=[bp])
            posb = s36()
            TT(posb[:, 0:32], bp[:, 0:32], cntb[:, :], ALU.add, R=[bp, cntb], W=[posb])
            STT(junk_d[:, 0:32], posb[:, 0:32], 1.0, M1[:, 0:32], ALU.mult, ALU.mult, R=[posb, M1], W=[junk_d, slots_f],
                accum=slots_f[:, gj, 0:1])
            STT(junk_d[:, 32:64], posb[:, 0:32], 1.0, M2[:, 0:32], ALU.mult, ALU.mult, R=[posb, M2], W=[junk_d, slots_f],
                accum=slots_f[:, gj, 1:2])
            TT(cntb[:, :], cntb[:, :], bp[:, 32:64], ALU.add, R=[cntb, bp], W=[cntb])
            okm = s36()
            TT(okm[:, 0:32], posb[:, 0:32], limb[:, :], ALU.is_lt, R=[posb, limb], W=[okm])
            ok2 = stat()
            for k_, Mk in ((0, M1), (1, M2)):
                STT(junk_d[:, 0:32], okm[:, 0:32], 1.0, Mk[:, 0:32], ALU.mult, ALU.mult, R=[okm, Mk], W=[junk_d, ok2],
                    accum=ok2[:, k_:k_ + 1])
                TS(ok2[:, 2 + k_:3 + k_], slots_f[:, gj, k_:k_ + 1], -float(NSLOT), ALU.add, R=[slots_f, ok2], W=[ok2])
                STT(slots_f[:, gj, k_:k_ + 1], ok2[:, 2 + k_:3 + k_], ok2[:, k_:k_ + 1], trashc[:, :], ALU.mult, ALU.add,
                    R=[ok2, trashc], W=[slots_f])
            TT(cw_all[:, gj, :], cw_all[:, gj, :], ok2[:, 0:2], ALU.mult, R=[cw_all, ok2], W=[cw_all])
            CP(slots_i[:, gj, :], slots_f[:, gj, :], R=[slots_f], W=[slots_i])
            if dbg:
                f = stat()
                CP(f[:, 0:2], slots_f[:, gj, :], R=[slots_f], W=[f])
                CP(f[:, 2:4], cw_all[:, gj, :], R=[cw_all], W=[f])
                DMA('sp', dbg_d["d_sl"][rows_d, :], f[:, 0:4], R=[f], W=[dbg_d["d_sl"]])
            for k_ in range(2):
                P.dma('pool', lambda e, gj=gj, k_=k_, h2=h2: e.indirect_dma_start(
                    out=Xg_d[:, :], out_offset=bass.IndirectOffsetOnAxis(ap=slots_i[:, gj, k_:k_ + 1], axis=0),
                    in_=h2[:, :], in_offset=None), _toks([h2, slots_i]), _toks([Xg_d]), nbytes=262144)
        w_done(2)

    phaseA(-1)
    for gi in range(NG):
        phaseA(gi)

    mark('A7.last')
    o_ = 0
    XgT2, xg2 = [], []
    for i in range(2):
        XgT2.append(Tile(arena[:, o_:o_ + 8 * CAP].rearrange("p (c t) -> p c t", c=8), f"XgT{i}"))
        o_ += 8 * CAP
        xg2.append(Tile(arena[:, o_:o_ + NB * 1024].rearrange("p (b d) -> p b d", b=NB), f"xg{i}"))
        o_ += NB * 1024
    hidT = Tile(arena[:, o_:o_ + 4 * CAP].rearrange("p (c t) -> p c t", c=4), "hidT")
    o_ += 4 * CAP
    wbB = list(wbufs)
    while o_ + 4096 <= AR_KT + AR_VA and len(wbB) < 9:
        wbB.append(Tile(arena[:, o_:o_ + 4096], f"wbB{len(wbB)}"))
        o_ += 4096
    assert o_ <= AR_KT + AR_VA
    NWB2 = len(wbB)
    bar = P.sb([128, 1], F32, "bar")
    P.op('dve', lambda e: e.memset(bar[:, :], 0.0), arena_toks,
         _toks(XgT2 + xg2 + [hidT, bar] + wbB[NWB:]))
    wsB = dict(load=0, use=0)
    gen_banks = Rot(banks[0:8])

    mT_toks = QT.toks
    stg = [
        [(hT.t[:, :, :].rearrange("p c t -> p (c t)").bitcast(F32), hT.toks, 0, 2048, 'dve'),
         (scr8[:, :].bitcast(F32), mT_toks, 2048, 4096, 'act')],
        [(base_oT[:, :], oT_tok[0] + oT_tok[1], 0, 2048, 'dve'),
         (base_sr[:, :], sr.toks, 2048, 3072, 'act'),
         (base_vg[:, :], vg.toks, 3072, 4096, 'act')],
    ]
    hw = dict(n=0)

    def wB_issue():
        k = wsB['load']
        if k < 96:
            buf = wbB[k % NWB2]
            if k % 5 in (1, 3):
                for (ap_, toks_, c0, c1, eng_) in stg[hw['n'] % 2]:
                    DMA('sp', ap_, wE_d[k][:, c0:c1], R=[], W=toks_)
                    CP(buf[:, c0:c1], ap_, R=toks_, W=[buf], eng=eng_)
                hw['n'] += 1
            else:
                DMA('pool', buf[:, :], wE_d[k], R=[], W=[buf])
            wsB['load'] += 1

    for _ in range(NWB2):
        wB_issue()

    def prep(e_):
        xg, XgT = xg2[e_ % 2], XgT2[e_ % 2]
        DMA('sp', xg[:, :, :], Xg_d[e_ * CAP:(e_ + 1) * CAP, :].rearrange("(b p) d -> p b d", p=128), R=[Xg_d], W=[xg])
        for b_ in range(NB):
            bk = gen_banks()
            bkb = bk[:, :].bitcast(BF16)
            for c in range(8):
                TR(bkb[:, c * 128:(c + 1) * 128], xg[:, b_, c * 128:(c + 1) * 128], ident_b, R=[xg, cmb], W=[bk])
            CP(XgT[:, :, b_ * 128:(b_ + 1) * 128], bkb[:, :].rearrange("p (c t) -> p c t", c=8), R=[bk], W=[XgT],
               eng=('act' if b_ % 2 == 0 else 'dve'))

    prep(0)
    for e_ in range(32):
        if e_ + 1 < 32:
            prep(e_ + 1)
        XgT = XgT2[e_ % 2]
        wg, wu, wd = (wbB[(wsB['use'] + i) % NWB2] for i in range(3))
        wg3 = wg[:, :].rearrange("p (c n) -> p c n", c=8)
        wu3 = wu[:, :].rearrange("p (c n) -> p c n", c=8)
        wd3 = wd[:, :].rearrange("p (c n) -> p c n", c=4)
        nch = -(-CAP // 512)
        csz = CAP // nch
        assert csz * nch == CAP
        for fc in range(4):
            for ch in range(nch):
                tsl = slice(ch * csz, (ch + 1) * csz)
                bg = gen_banks()
                for c in range(8):
                    MM(bg[:, 0:csz], lhsT=wg3[:, c, fc * 128:(fc + 1) * 128], rhs=XgT[:, c, tsl], start=(c == 0), stop=(c == 7),
                       R=[wg, XgT], W=[bg])
                bu = gen_banks()
                for c in range(8):
                    MM(bu[:, 0:csz], lhsT=wu3[:, c, fc * 128:(fc + 1) * 128], rhs=XgT[:, c, tsl], start=(c == 0), stop=(c == 7),
                       R=[wu, XgT], W=[bu])
                s_ = frow()
                ACT(s_[:, 0:csz], bg[:, 0:csz], AF.Silu, R=[bg], W=[s_])
                TT(hidT[:, fc, tsl], bu[:, 0:csz], s_[:, 0:csz], ALU.mult, R=[bu, s_], W=[hidT])
        for b_ in range(NB):
            y = frow()
            for half in range(2):
                bk = gen_banks()
                for fc in range(4):
                    MM(bk[:, :], lhsT=hidT[:, fc, b_ * 128:(b_ + 1) * 128], rhs=wd3[:, fc, half * 512:(half + 1) * 512],
                       start=(fc == 0), stop=(fc == 3), R=[hidT, wd], W=[bk])
                CP(y[:, half * 512:(half + 1) * 512], bk[:, :], R=[bk], W=[y], eng=('act' if half == 0 else 'dve'))
            DMA('sp', Yg_d[e_ * CAP + b_ * 128:e_ * CAP + (b_ + 1) * 128, :], y[:, :], R=[y], W=[Yg_d])
        for _ in range(3):
            wsB['use'] += 1
            wB_issue()

    mark('experts')
    for gj in range(NT):
        rows_d = slice(gj * 128, (gj + 1) * 128)
        y1, y2, u = frow(), frow(), frow()
        for k_, yt in ((0, y1), (1, y2)):
            P.dma('pool', lambda e, gj=gj, k_=k_, yt=yt: e.indirect_dma_start(
                out=yt[:, :], out_offset=None, in_=Yg_d[:, :],
                in_offset=bass.IndirectOffsetOnAxis(ap=slots_i[:, gj, k_:k_ + 1], axis=0)),
                _toks([Yg_d, slots_i]), _toks([yt]), nbytes=524288)
        DMA('sp', u[:, :], U_d[rows_d, :], R=[U_d], W=[u])
        STT(u[:, :], y1[:, :], cw_all[:, gj, 0:1], u[:, :], ALU.mult, ALU.add, R=[y1, cw_all, u], W=[u])
        STT(u[:, :], y2[:, :], cw_all[:, gj, 1:2], u[:, :], ALU.mult, ALU.add, R=[y2, cw_all, u], W=[u])
        DMA('sp', out_d[rows_d, :], u[:, :], R=[u], W=[out_d])

    mark('combine')
    P.build()
    return nc, P.stats


def host_prep(inp, T):
    f = lambda a: np.ascontiguousarray(np.asarray(a, dtype=np.float32))
    w_in = f(inp['w_in'])[0]
    o = np.cumsum([0, 512, 512, 512, 256, 256, 512, 512, 16, 2048])
    da_q, da_k, da_v = w_in[:, o[0]:o[1]], w_in[:, o[1]:o[2]], w_in[:, o[2]:o[3]]
    gq, gk, gv, gr, gg = (w_in[:, o[3]:o[4]], w_in[:, o[4]:o[5]], w_in[:, o[5]:o[6]], w_in[:, o[6]:o[7]], w_in[:, o[7]:o[8]])
    gates = w_in[:, o[8]:o[9]]
    z = np.zeros((1024, 512 - 272), np.float32)
    blocks = [np.concatenate([gk, gg, z], 1), np.concatenate([gq, gk], 1), da_k, da_v, da_q, gv, gr]
    for gb in range(4):
        blocks.append(np.concatenate([gates[:, gb * 256:(gb + 1) * 256], gates[:, 1024 + gb * 256:1024 + (gb + 1) * 256]], 1))

    def blk8(w):
        return w.reshape(8, 128, 512).transpose(1, 0, 2).reshape(128, 4096)

    def blk4(w):
        return w.reshape(4, 128, 1024).transpose(1, 0, 2).reshape(128, 4096)
    wA = [blk8(b) for b in blocks]
    wbr = f(inp['w_branch'])[0]
    brp = wbr.reshape(2, 4, 128, 4, 256).transpose(3, 2, 0, 1, 4).reshape(2, 2, 128, 2048).transpose(0, 2, 1, 3).reshape(2, 128, 4096)
    wA += [brp[0], brp[1]]
    wo = f(inp['w_out'])[0]
    wA += [blk8(wo[:, 0:512]), blk8(wo[:, 512:1024])]
    wA = np.ascontiguousarray(np.stack(wA, 0))
    weg, weu, wed = f(inp['w_exp_gate'])[0], f(inp['w_exp_up'])[0], f(inp['w_exp_down'])[0]
    wE = np.empty((96, 128, 4096), np.float32)
    wE[0::3] = weg.reshape(32, 8, 128, 512).transpose(0, 2, 1, 3).reshape(32, 128, 4096)
    wE[1::3] = weu.reshape(32, 8, 128, 512).transpose(0, 2, 1, 3).reshape(32, 128, 4096)
    wE[2::3] = wed.reshape(32, 4, 128, 1024).transpose(0, 2, 1, 3).reshape(32, 128, 4096)
    wr = np.concatenate([f(inp['w_router_group'])[0], f(inp['w_router_expert'])[0].reshape(1024, 32)], 1)
    wr = np.ascontiguousarray(wr.reshape(8, 128, 36).transpose(1, 0, 2).reshape(128, 288))
    pos = np.arange(T + 16, dtype=np.float32)
    inv = np.power(np.float32(10000.0), -np.arange(32, dtype=np.float32) * 2.0 / 64).astype(np.float32)
    ang = pos[:, None] * inv[None, :]
    rope = np.concatenate([np.cos(ang), np.sin(ang), -np.sin(ang)], 1).astype(np.float32)
    s = np.arange(128)[:, None]
    t = np.arange(128)[None, :]
    cmask = np.concatenate([(s == t), (s <= t), (s < t), np.where(s <= t, 0.0, -30000.0),
                            (s <= t) * (-1.0 / 16), (s > t) * (-1.0 / 16)], 1).astype(np.float32)
    gmixT = np.ascontiguousarray(f(inp['g_mix_norm'])[0].reshape(8, 128).T)
    bmgT = np.ascontiguousarray(f(inp['b_merge_gate'])[0].reshape(16, 128).T)
    vecs = np.zeros((1, 1956), np.float32)
    vecs[0, 0:1024] = f(inp['g_ffn_norm'])[0]
    vecs[0, 1024:1088] = f(inp['g_q_norm'])[0]
    vecs[0, 1088:1152] = f(inp['g_k_norm'])[0]
    vecs[0, 1152:1280] = f(inp['g_diff_subln'])[0]
    vecs[0, 1280:1408] = f(inp['g_gla_norm'])[0]
    vecs[0, 1408:1664] = f(inp['b_gla_gate'])[0]
    vecs[0, 1664:1668] = f(inp['b_router_group'])[0]
    vecs[0, 1668:1700] = f(inp['b_router_expert'])[0].reshape(32)
    vecs[0, 1700:1764] = f(inp['lambda_q1'])[0]
    vecs[0, 1764:1828] = f(inp['lambda_k1'])[0]
    vecs[0, 1828:1892] = f(inp['lambda_q2'])[0]
    vecs[0, 1892:1956] = f(inp['lambda_k2'])[0]
    shared = dict(meta=f(inp['meta_tokens']), wA=wA, wE=wE, wr=wr, rope=rope, cmask=cmask, gmixT=gmixT, bmgT=bmgT,
                  vecs=vecs, wup=f(inp['w_gla_gate_up'])[0])
    return shared


_CACHE = {}


def run(inp, T, CAP, ncores, dbg=False):
    key = (T, CAP, dbg)
    if key not in _CACHE:
        _CACHE[key] = build_program(T, CAP, dbg)
    nc, stats = _CACHE[key]
    shared = host_prep(inp, T)
    shared['ebase'] = (np.arange(32, dtype=np.float32) * CAP).reshape(1, 32)
    x = np.asarray(inp['x'], dtype=np.float32)
    in_maps = []
    for b in range(ncores):
        d = dict(shared)
        d['x'] = np.ascontiguousarray(x[b])
        in_maps.append(d)
    res = run_bass_kernel_spmd(nc, in_maps, core_ids=list(range(ncores)))
    return res, stats


def kernel(**inputs):
    res, _ = run(inputs, 4096, 512, 8)
    return np.stack([np.asarray(r["out"], dtype=np.float32) for r in res.results], 0)
```

```python
import math
import numpy as np
import ml_dtypes
from contextlib import ExitStack
import concourse.bass as bass
import concourse.mybir as mybir
from concourse.bass_utils import run_bass_kernel_spmd

F32 = mybir.dt.float32
BF16 = mybir.dt.bfloat16
I32 = mybir.dt.int32
AF = mybir.ActivationFunctionType
ALU = mybir.AluOpType
AX = mybir.AxisListType

ND_SEM = 20
COMPUTE = ('pe', 'act', 'dve', 'pool')
EPS = 1e-6
N_META = 16


class Tok:
    __slots__ = ('name', 'w', 'r', 'ps')

    def __init__(self, name=''):
        self.name = name
        self.ps = False
        self.w = None
        self.r = []


class Tile:
    def __init__(self, t, name, toks=None):
        self.t = t
        self.toks = toks if toks is not None else [Tok(name)]

    def __getitem__(self, k):
        return self.t[k]


def _toks(xs):
    out = []
    for x in xs:
        if isinstance(x, Tile):
            out.extend(x.toks)
        elif isinstance(x, Tok):
            out.append(x)
        else:
            out.extend(_toks(x))
    return tuple(out)


class Prog:
    def __init__(self, nc):
        self.nc = nc
        self.ops = []
        self.costs = []
        self.reorder_on = True
        self.trace_sim = None
        self.use_blevel = True
        self.marks = []
        self.es = ExitStack()
        self.n_alloc = 0

    def sb(self, shape, dt, name=None):
        self.n_alloc += 1
        name = "s_" + (name or f"sb{self.n_alloc}")
        t = self.es.enter_context(self.nc.sbuf_tensor(name, list(shape), dt))
        return Tile(t, name)

    def ps(self, shape, dt, name=None):
        self.n_alloc += 1
        name = name or f"ps{self.n_alloc}"
        t = self.es.enter_context(self.nc.psum_tensor(name, list(shape), dt))
        tl = Tile(t, name)
        tl.toks[0].ps = True
        return tl

    def dram(self, name, shape, dt, kind="Internal"):
        t = self.nc.dram_tensor(name, list(shape), dt, kind=kind)
        return Tile(t, name)

    def op(self, eng, fn, reads=(), writes=(), cost=0.3):
        r, w = _toks(reads), _toks(writes)
        w = w + tuple(t for t in r if t.ps)
        r = tuple(t for t in r if not t.ps)
        self.ops.append((eng, fn, r, w, False))
        self.costs.append(cost)

    def dma(self, q, fn, reads=(), writes=(), nbytes=65536):
        self.ops.append((q, fn, _toks(reads), _toks(writes), True))
        self.costs.append(nbytes)

    def reorder(self, deps):
        import heapq
        ops, costs = self.ops, self.costs
        n = len(ops)
        engs = ['pe', 'act', 'dve', 'pool', 'sp']
        succ = [[] for _ in range(n)]
        indeg = [0] * n
        for i in range(n):
            indeg[i] = len(deps[i])
            for j in deps[i]:
                succ[j].append(i)
        per = {e: [] for e in engs}
        for i, o in enumerate(ops):
            per[o[0]].append(i)
        dur_est = [(costs[i] / 250e3 + 2.0) if ops[i][4] else costs[i] for i in range(n)]
        blevel = [0.0] * n
        for i in range(n - 1, -1, -1):
            m_ = 0.0
            for k in succ[i]:
                if blevel[k] > m_:
                    m_ = blevel[k]
            blevel[i] = dur_est[i] + m_
        ptr = {e: 0 for e in engs}
        done = [False] * n
        ready = [0.0] * n
        te = {e: 0.0 for e in engs}
        dma_free = 0.0
        qfree = {e: 0.0 for e in engs}
        order = {e: [] for e in engs}
        W = {'pe': 256, 'act': 64, 'dve': 64, 'pool': 16, 'sp': 24}
        left = n
        BW = 180e3
        while left:
            best = None
            for e in engs:
                lst = per[e]
                p = ptr[e]
                while p < len(lst) and done[lst[p]]:
                    p += 1
                ptr[e] = p
                seen = 0
                q = p
                while q < len(lst) and seen < W[e]:
                    i = lst[q]
                    q += 1
                    if done[i]:
                        continue
                    seen += 1
                    if indeg[i]:
                        continue
                    st_ = max(te[e], ready[i])
                    key = (st_, -blevel[i] if self.use_blevel else i, i)
                    if best is None or key < best[0]:
                        best = (key, e, i)
                    if st_ <= te[e] and not self.use_blevel:
                        break
            assert best is not None, "scheduler deadlock"
            (st_, _, _), e, i = best
            te_before = te[e]
            if ops[i][4]:
                issue = 0.9 if e == 'pool' else 0.12
                t0 = max(st_ + issue, dma_free, qfree[e])
                dur = costs[i] / (215e3 if e == 'pool' else 300e3)
                qfree[e] = t0 + dur
                dma_free = t0 + costs[i] / 340e3
                fin = t0 + dur + 1.8
                te[e] = st_ + issue
            else:
                te[e] = st_ + costs[i]
                fin = te[e] + 0.12
            done[i] = True
            left -= 1
            order[e].append(i)
            if self.trace_sim is not None:
                self.trace_sim.append((i, e, st_, fin, te_before, ready[i]))
            for k in succ[i]:
                indeg[k] -= 1
                if ready[k] < fin:
                    ready[k] = fin
        self.sim_time = max(te.values())
        return order

    def build(self):
        nc = self.nc
        ops = self.ops
        n = len(ops)
        deps = [None] * n
        for i, (eng, fn, reads, writes, isdma) in enumerate(ops):
            d = set()
            for b in reads:
                if b.w is not None:
                    d.add(b.w)
            for b in writes:
                if b.w is not None:
                    d.add(b.w)
                d.update(b.r)
            d.discard(i)
            deps[i] = d
            for b in reads:
                b.r.append(i)
            for b in writes:
                b.w = i
                b.r = []
        engs = ['pe', 'act', 'dve', 'pool', 'sp']
        self.deps_saved = deps
        if self.reorder_on:
            stream = self.reorder(deps)
        else:
            stream = {e: [] for e in engs}
            for i, o in enumerate(ops):
                stream[o[0]].append(i)
        pos = [0] * n
        for e in engs:
            for p_, i in enumerate(stream[e]):
                pos[i] = p_
        needed = [False] * n
        real = [None] * n
        for i in range(n):
            e = ops[i][0]
            rd = []
            for j in deps[i]:
                ej, isd = ops[j][0], ops[j][4]
                if not isd and ej == e:
                    if e in ('pe', 'sp'):
                        continue
                    assert pos[i] > pos[j]
                rd.append(j)
                needed[j] = True
            real[i] = rd
        cnt = [0] * n
        dmaidx = [0] * n
        ccount = {e: 0 for e in engs}
        dcount = {e: 0 for e in engs}
        for e in engs:
            for i in stream[e]:
                if ops[i][4]:
                    dmaidx[i] = dcount[e]
                    dcount[e] += 1
                elif needed[i]:
                    ccount[e] += 1
                    cnt[i] = ccount[e]
        es = self.es
        csem = {e: es.enter_context(nc.semaphore(f"c_{e}")) for e in COMPUTE}
        dsem = {}
        for e in engs:
            if dcount[e]:
                dsem[e] = [es.enter_context(nc.semaphore(f"d_{e}{k}"))
                           for k in range(min(ND_SEM, dcount[e]))]
        self.stats = dict(n_ops=n, per_eng={e: len(stream[e]) for e in engs},
                          incs=dict(ccount), dmas=dict(dcount))

        def dma_sem_val(j):
            e = ops[j][0]
            k = dmaidx[j]
            return dsem[e][k % ND_SEM], 16 * (k // ND_SEM + 1)

        block = es.enter_context(nc.Block())
        nwaits = {}

        def make(e):
            def body(engine):
                waited = {}
                nw = 0
                for i in stream[e]:
                    _, fn, _, _, isdma = ops[i]
                    need = {}
                    for j in real[i]:
                        if ops[j][4]:
                            s, v = dma_sem_val(j)
                        else:
                            s, v = csem[ops[j][0]], cnt[j]
                        key = id(s)
                        if key not in need or need[key][1] < v:
                            need[key] = (s, v)
                    if isdma:
                        k = dmaidx[i]
                        if k >= ND_SEM:
                            s = dsem[e][k % ND_SEM]
                            v = 16 * (k // ND_SEM)
                            key = id(s)
                            if key not in need or need[key][1] < v:
                                need[key] = (s, v)
                    todo = []
                    for key, (s, v) in need.items():
                        if waited.get(key, 0) >= v:
                            continue
                        waited[key] = v
                        todo.append((s, v))
                    n_alone = len(todo) if isdma else max(0, len(todo) - 1)
                    for (s, v) in todo[:n_alone]:
                        engine.wait_ge(s, v)
                        nw += 1
                    ins = fn(engine)
                    if todo and not isdma:
                        ins._wait_ge(todo[-1][0], todo[-1][1])
                    if isdma:
                        s, _ = dma_sem_val(i)
                        ins.then_inc(s, 16)
                    elif needed[i]:
                        ins.then_inc(csem[e], 1)
                if e == 'sp':
                    for q in engs:
                        if dcount[q]:
                            for k in range(max(0, dcount[q] - ND_SEM), dcount[q]):
                                engine.wait_ge(dsem[q][k % ND_SEM], 16 * (k // ND_SEM + 1))
                nwaits[e] = nw
            return body

        block.sync(make('sp'))
        if stream['pe']:
            block.tensor(make('pe'))
        if stream['act']:
            block.scalar(make('act'))
        if stream['dve']:
            block.vector(make('dve'))
        if stream['pool']:
            block.gpsimd(make('pool'))
        self.stats['waits'] = nwaits
        self.stats['sim_us'] = getattr(self, 'sim_time', None)
        es.close()
        return nc


class Rot:
    def __init__(self, tiles):
        self.tiles = tiles
        self.i = 0

    def __call__(self):
        t = self.tiles[self.i % len(self.tiles)]
        self.i += 1
        return t


def build_program(T, CAP, dbg=False):
    assert T % 512 == 0 and CAP % 128 == 0
    NG = T // 512
    NT = T // 128
    NB = CAP // 128
    NSLOT = 32 * CAP
    LAM_INIT = 0.8 - 0.6 * math.exp(-0.3 * 0)
    nc = bass.Bass("TRN2", target_bir_lowering=False)
    P = Prog(nc)

    def din(name, shape, dt=F32):
        return P.dram(name, shape, dt, kind="ExternalInput")
    x_d = din("x", [T, 1024])
    meta_d = din("meta", [16, 1024])
    wA_d = din("wA", [15, 128, 4096])
    wE_d = din("wE", [96, 128, 4096])
    wr_d = din("wr", [128, 8 * 36])
    rope_d = din("rope", [T + 16, 96])
    cm_d = din("cmask", [128, 6 * 128])
    gmixT_d = din("gmixT", [128, 8])
    bmgT_d = din("bmgT", [128, 16])
    vec_d = din("vecs", [1, 1956])
    wup_d = din("wup", [16, 256])
    ebase_d = din("ebase", [1, 32])
    out_d = P.dram("out", [T, 1024], F32, kind="ExternalOutput")
    Xg_d = P.dram("Xg", [NSLOT + 128, 1024], BF16)
    Yg_d = P.dram("Yg", [NSLOT + 128, 1024], F32)
    U_d = P.dram("U", [T, 1024], F32)
    dbg_d = {}
    if dbg:
        for nm, shp in (("d_oa", [T, 512]), ("d_ob", [T, 512]), ("d_u", [T, 1024]), ("d_lg", [T, 36]),
                        ("d_sl", [T, 4]), ("d_st", [128, 8]), ("d_o", [128, 128]), ("d_acc", [128, 258])):
            dbg_d[nm] = P.dram(nm, shp, F32, kind="ExternalOutput")

    V_GFFN, V_GQ, V_GK, V_GSUB, V_GGLA, V_BGATE, V_BR, V_LAM = 0, 1024, 1088, 1152, 1280, 1408, 1664, 1700

    vecs = P.sb([128, 1956], F32, "vecs")
    cm = P.sb([128, 6 * 128], F32, "cm")
    ident_f = cm[:, 0:128]
    tri01_f = cm[:, 128:256]
    slt01_f = cm[:, 256:384]
    triS_f = cm[:, 512:640]
    sgtS_f = cm[:, 640:768]
    cmb = P.sb([128, 4 * 128], BF16, "cmb")
    ident_b = cmb[:, 0:128]
    maskb_b = cmb[:, 384:512]
    ones_f = P.sb([128, 128], F32, "ones_f")
    gmixT = P.sb([128, 8], F32, "gmixT")
    bmgT = P.sb([128, 16], F32, "bmgT")
    nbmgT = P.sb([128, 16], F32, "nbmgT")
    wr_sb = P.sb([128, 8 * 36], F32, "wr_sb")
    wup_f = P.sb([16, 256], F32, "wup_f")
    wup_b = P.sb([16, 256], BF16, "wup_b")
    gsub_s = P.sb([128, 128], F32, "gsub_s")
    neglam = P.sb([128, 1], F32, "neglam")
    lamt = P.sb([128, 4], F32, "lamt")
    junk_f = P.sb([128, 512], F32, "junk_f")
    junk_b = P.sb([128, 1024], BF16, "junk_b")
    junk_d = P.sb([128, 64], F32, "junk_d")
    junk_d2 = P.sb([128, 128], F32, "junk_d2")
    cntb = P.sb([128, 32], F32, "cntb")
    limb = P.sb([128, 32], F32, "limb")
    trashc = P.sb([128, 1], F32, "trashc")

    AR_KT = 4 * (T + 16)
    AR_VA = (NT + 1) * 4 * 129
    arena = P.sb([128, AR_KT + AR_VA], BF16, "arena")
    KT = {}
    VA = {}
    off = 0
    for g in [-1] + list(range(NG)):
        ntok = 16 if g < 0 else 512
        KT[g] = Tile(arena[:, off:off + 4 * ntok].rearrange("p (h t) -> p h t", h=4), f"KT{g}")
        off += 4 * ntok
    for g in [-1] + list(range(NG)):
        ntl = 1 if g < 0 else 4
        VA[g] = Tile(arena[:, off:off + ntl * 516].rearrange("p (j h e) -> p j h e", j=ntl, h=4), f"VA{g}")
        off += ntl * 516
    arena_toks = [t for g in KT for t in KT[g].toks] + [t for g in VA for t in VA[g].toks]

    NWB = 4
    wbufs = [P.sb([128, 4096], BF16, f"wb{i}") for i in range(NWB)]
    hT = P.sb([128, 8, 512], BF16, "hT")
    frow = Rot([P.sb([128, 1024], F32, f"frow{i}") for i in range(5)])
    ropet = P.sb([128, 4, 96], F32, "ropet")
    scr8 = P.sb([128, 4096], BF16, "scr8")
    QT = Tile(scr8[:, :].rearrange("p (h m t) -> p h m t", h=4, m=2), "QT")
    scrG = P.sb([128, 2048], BF16, "scrG")
    qtT = Tile(scrG[:, 0:1024].rearrange("p (a t) -> p a t", a=2), "qtT")
    ktT = Tile(scrG[:, 1024:2048].rearrange("p (a t) -> p a t", a=2), "ktT")
    mT = Tile(scr8[:, :].rearrange("p (c t) -> p c t", c=8), "mT", toks=QT.toks)
    gT = P.sb([16, 512], BF16, "gT")
    l_tok = P.sb([128, 4, 256], F32, "l_tok")
    ebT = P.sb([128, 2, 512], BF16, "ebT")
    enbT = P.sb([128, 2, 512], BF16, "enbT")
    eblast = P.sb([128, 2, 4], F32, "eblast")
    ek = Rot([P.sb([128, 256], F32, f"ek{i}") for i in range(2)])
    khat = P.sb([128, 4, 256], BF16, "khat")
    base_vg = P.sb([128, 1024], F32, "base_vg")
    vg = Tile(base_vg[:, :].bitcast(BF16).rearrange("p (j n) -> p j n", j=4), "vg", toks=[Tok("vg0"), Tok("vg1")])
    base_sr = P.sb([128, 1024], F32, "base_sr")
    sr = Tile(base_sr[:, :].bitcast(BF16).rearrange("p (j n) -> p j n", j=4), "sr", toks=[Tok("sr0"), Tok("sr1")])
    state_f = P.sb([128, 2, 128], F32, "state_f")
    state_b = P.sb([128, 2, 128], BF16, "state_b")
    base_oa = P.sb([128, 1024], F32, "base_oa")
    oa_b = base_oa[:, :].bitcast(BF16)
    oa_tok = [Tile(oa_b[:, i * 512:(i + 1) * 512], f"oa_tok{i}") for i in range(4)]
    ob_tiles = [P.sb([128, 512], BF16, f"ob_tok{i}") for i in range(2)]
    ob_tok = Rot(ob_tiles)
    base_oT = P.sb([128, 2048], F32, "base_oT")
    oT = Tile(base_oT[:, :].bitcast(BF16).rearrange("p (n w t) -> p n w t", n=2, w=4), "oT")
    oT_tok = [[Tok(f"oT{n}_{j}") for j in range(4)] for n in range(2)]
    base_PT = P.sb([128, 1024], F32, "base_PT")
    PT_b = base_PT[:, :].bitcast(BF16)
    PT_tiles = [Tile(PT_b[:, i * 512:(i + 1) * 512], f"PT{i}") for i in range(4)]
    PT = Rot(PT_tiles)
    AT = Rot([P.sb([128, 128], BF16, f"AT{i}") for i in range(2)])
    qn = Rot([Tile(base_PT[:, i * 512:(i + 1) * 512], f"qn{i}", toks=PT_tiles[2 * i].toks + PT_tiles[2 * i + 1].toks)
              for i in range(2)])
    qr = Rot([Tile(base_oa[:, i * 512:(i + 1) * 512], f"qr{i}", toks=oa_tok[2 * i].toks + oa_tok[2 * i + 1].toks)
              for i in range(2)])
    qb = Rot(ob_tiles)
    hb = Rot([P.sb([128, 1024], BF16, f"hb{i}") for i in range(1)])
    m0 = Rot([Tile(base_sr[:, i * 512:(i + 1) * 512], f"m0{i}", toks=[sr.toks[i]]) for i in range(2)]
             + [Tile(base_vg[:, i * 512:(i + 1) * 512], f"m0v{i}", toks=[vg.toks[i]]) for i in range(2)])
    o_sb = Rot([P.sb([128, 128], F32, f"o_sb{i}") for i in range(3)])
    oT_b = base_oT[:, :].bitcast(BF16)
    h2b = Rot([Tile(oT_b[:, 2048 + i * 1024:2048 + (i + 1) * 1024], f"h2b{i}", toks=oT_tok[1]) for i in range(2)])
    h2T = Tile(base_oT[:, 0:1024].rearrange("p (c t) -> p c t", c=8), "h2T", toks=oT_tok[0])
    st = Rot([P.sb([128, 8], F32, f"st{i}") for i in range(16)])
    s36 = Rot([P.sb([128, 36], F32, f"s36{i}") for i in range(12)])
    slots_i = P.sb([128, NT, 2], I32, "slots_i")
    slots_f = P.sb([128, NT, 2], F32, "slots_f")
    cw_all = P.sb([128, NT, 2], F32, "cw_all")

    banks = [P.ps([128, 512], F32, f"bank{i}") for i in range(8)]
    gen_banks = Rot(banks[0:4])
    acc_tile = {}
    acc_tok = {}
    for m in range(2):
        for jq in range(4):
            acc_tok[(m, jq)] = banks[4 + jq].toks[0]
            acc_tile[(m, jq)] = banks[4 + jq][:, m * 129:(m + 1) * 129]

    def fsz(ap):
        n_ = 1
        for d_ in ap.shape[1:]:
            n_ *= d_
        return n_

    def MM(out, lhsT, rhs, start=True, stop=True, R=(), W=(), sgc=False):
        c_ = (0.08 + fsz(out) / 2700.0) * (3.0 if lhsT.dtype == F32 else 1.0) * (2.0 if lhsT.shape[0] == 64 else 1.0)
        if sgc:
            P.op('pe', lambda e: e.matmul(out, lhsT=lhsT, rhs=rhs, start=start, stop=stop, skip_group_check=True), R, W, cost=c_)
        else:
            P.op('pe', lambda e: e.matmul(out, lhsT=lhsT, rhs=rhs, start=start, stop=stop), R, W, cost=c_)

    def TR(out, in_, ident, R=(), W=()):
        c_ = (0.08 + fsz(out) / 2700.0) * (3.0 if in_.dtype == F32 else 1.0)
        P.op('pe', lambda e: e.transpose(out, in_, ident), R, W, cost=c_)

    def ACT(out, in_, func, R=(), W=(), bias=None, scale=None, accum=None):
        kw = {}
        if bias is not None:
            kw['bias'] = bias
        if scale is not None:
            kw['scale'] = scale
        if accum is not None:
            kw['accum_out'] = accum
        P.op('act', lambda e: e.activation(out=out, in_=in_, func=func, **kw), R, W, cost=(fsz(out) + 190) / 1200.0)

    def TT(out, in0, in1, op, R=(), W=(), eng='dve'):
        P.op(eng, lambda e: e.tensor_tensor(out=out, in0=in0, in1=in1, op=op), R, W, cost=(fsz(out) + 120) / 960.0)

    def TS(out, in0, s1, op0, R=(), W=(), s2=None, op1=None, eng='dve'):
        if op1 is None:
            P.op(eng, lambda e: e.tensor_scalar(out=out, in0=in0, scalar1=s1, scalar2=None, op0=op0), R, W,
                 cost=(fsz(out) * 0.6 + 120) / 960.0)
        else:
            P.op(eng, lambda e: e.tensor_scalar(out=out, in0=in0, scalar1=s1, scalar2=s2, op0=op0, op1=op1), R, W,
                 cost=(fsz(out) * 0.6 + 120) / 960.0)

    def STT(out, in0, scalar, in1, op0, op1, R=(), W=(), accum=None, eng='dve'):
        if accum is None:
            P.op(eng, lambda e: e.scalar_tensor_tensor(out=out, in0=in0, scalar=scalar, in1=in1, op0=op0, op1=op1), R, W,
                 cost=(fsz(out) + 120) / 960.0)
        else:
            P.op(eng, lambda e: e.scalar_tensor_tensor(out=out, in0=in0, scalar=scalar, in1=in1, op0=op0, op1=op1,
                                                       accum_out=accum), R, W, cost=(fsz(out) + 120) / 960.0)

    def CP(out, in_, R=(), W=(), eng='dve'):
        if eng == 'act':
            P.op('act', lambda e: e.copy(out=out, in_=in_), R, W, cost=(fsz(out) + 300) / 1200.0)
        else:
            P.op(eng, lambda e: e.tensor_copy(out=out, in_=in_), R, W, cost=(fsz(out) * 0.6 + 120) / 960.0)

    def RED(out, in_, op, R=(), W=()):
        P.op('dve', lambda e: e.tensor_reduce(out=out, in_=in_, axis=AX.X, op=op), R, W, cost=(fsz(in_) + 120) / 960.0)

    def RECIP(out, in_, R=(), W=()):
        P.op('dve', lambda e: e.reciprocal(out=out, in_=in_), R, W, cost=(fsz(out) * 5.5 + 150) / 960.0)

    def MEMSET(ap, val, W=(), eng='dve'):
        P.op(eng, lambda e: e.memset(ap, val), (), W, cost=(fsz(ap) * 0.5 + 100) / 960.0)

    def DMA(q, out, in_, R=(), W=()):
        nb_ = out.shape[0] * fsz(out) * (4 if in_.dtype == F32 else 2)
        P.dma(q, lambda e: e.dma_start(out=out, in_=in_), R, W, nbytes=nb_)

    def rms_scale(ssq, n, tp):
        pass

    wAc_d = P.dram("wAc", [15, 128, 4096], BF16)
    wAc_tok = [Tok(f"wAc{i}") for i in range(15)]
    wsrc = []
    for g in [-1] + list(range(NG)):
        if g < 0:
            wsrc += [(g, i) for i in (0, 2, 3, 5)]
        else:
            wsrc += [(g, i) for i in (0, 1, 2, 3, 4, 5, 6, 11, 7, 8, 12, 9, 10, 13, 14)]
    wstate = dict(load=0, use=0)

    def w_issue():
        k = wstate['load']
        if k < len(wsrc):
            buf = wbufs[k % NWB]
            g, i = wsrc[k]
            if g <= 0:
                DMA('pool', buf[:, :], wA_d[i], R=[], W=[buf])
                if g == 0 and NG > 1:
                    DMA('sp', wAc_d[i], buf[:, :], R=[buf], W=[wAc_tok[i]])
            else:
                DMA('sp', buf[:, :], wAc_d[i], R=[wAc_tok[i]], W=[buf])
            wstate['load'] += 1

    def w_get(off=0):
        return wbufs[(wstate['use'] + off) % NWB]

    def w_done(n=1):
        for _ in range(n):
            wstate['use'] += 1
            w_issue()

    DMA('sp', vecs[:, :], vec_d[0:1, :].partition_broadcast(128), W=[vecs])
    DMA('sp', cm[:, :], cm_d[:, :], W=[cm])
    DMA('sp', gmixT[:, :], gmixT_d[:, :], W=[gmixT])
    DMA('sp', bmgT[:, :], bmgT_d[:, :], W=[bmgT])
    DMA('sp', wr_sb[:, :], wr_d[:, :], W=[wr_sb])
    DMA('sp', wup_f[:, :], wup_d[:, :], W=[wup_f])
    DMA('sp', cntb[:, :], ebase_d[0:1, :].partition_broadcast(128), W=[cntb])
    DMA('sp', limb[:, :], ebase_d[0:1, :].partition_broadcast(128), W=[limb])
    TS(limb[:, :], limb[:, :], float(CAP), ALU.add, R=[limb], W=[limb])
    MEMSET(trashc[:, :], float(NSLOT), W=[trashc])
    for _ in range(NWB):
        w_issue()
    MEMSET(base_oT[:, :], 0.0, W=oT_tok[0] + oT_tok[1])
    CP(cmb[:, :], cm[:, 0:512], R=[cm], W=[cmb])
    TS(nbmgT[:, :], bmgT[:, :], -1.0, ALU.mult, R=[bmgT], W=[nbmgT])
    CP(wup_b[:, :], wup_f[:, :], R=[wup_f], W=[wup_b])
    MEMSET(ones_f[:, :], 1.0, W=[ones_f])
    MEMSET(state_f[:, :, :], 0.0, W=[state_f])
    MEMSET(state_b[:, :, :], 0.0, W=[state_b])
    MEMSET(arena[:, AR_KT:AR_KT + AR_VA], 1.0, W=[VA[g] for g in VA])
    for i in range(2):
        STT(junk_d[:, 0:64], vecs[:, V_LAM + 128 * i:V_LAM + 128 * i + 64], 1.0,
            vecs[:, V_LAM + 128 * i + 64:V_LAM + 128 * i + 128], ALU.mult, ALU.mult,
            R=[vecs], W=[junk_d, lamt], accum=lamt[:, i:i + 1])
    ACT(lamt[:, 2:4], lamt[:, 0:2], AF.Exp, R=[lamt], W=[lamt])
    TT(lamt[:, 0:1], lamt[:, 3:4], lamt[:, 2:3], ALU.subtract, R=[lamt], W=[lamt])
    TS(neglam[:, :], lamt[:, 0:1], -LAM_INIT, ALU.add, R=[lamt], W=[neglam])
    TS(gsub_s[:, :], vecs[:, V_GSUB:V_GSUB + 128], 1.0 - LAM_INIT, ALU.mult, R=[vecs], W=[gsub_s])

    def stat():
        return st()

    def rstd_from_ssq(ss_tile, ncol, n, tp):
        ACT(ss_tile[:tp, 0:ncol], ss_tile[:tp, 0:ncol], AF.Ln, R=[ss_tile], W=[ss_tile], bias=EPS, scale=1.0 / n)
        ACT(ss_tile[:tp, 0:ncol], ss_tile[:tp, 0:ncol], AF.Exp, R=[ss_tile], W=[ss_tile], scale=-0.5)

    def qk_norm_rope(ps_bank, tp, j, gain_off, dst_T, dst_cols):
        s8 = stat()
        ACT(junk_f[:tp, 0:512], ps_bank[:tp, :], AF.Square, R=[ps_bank], W=[junk_f])
        RED(s8[:tp, 0:8], junk_f[:tp, 0:512].rearrange("p (g d) -> p g d", d=64), ALU.add, R=[junk_f], W=[s8])
        rstd_from_ssq(s8, 8, 64, tp)
        a = qn()
        TT(a[:tp, :].rearrange("p (g d) -> p g d", d=64), ps_bank[:tp, :].rearrange("p (g d) -> p g d", d=64),
           s8[:tp, 0:8].unsqueeze(2).to_broadcast([tp, 8, 64]), ALU.mult, R=[ps_bank, s8], W=[a])
        TT(a[:tp, :].rearrange("p (g d) -> p g d", d=64), a[:tp, :].rearrange("p (g d) -> p g d", d=64),
           vecs[:tp, gain_off:gain_off + 64].unsqueeze(1).to_broadcast([tp, 8, 64]), ALU.mult, R=[a, vecs], W=[a])
        r = qr()
        a16 = a[:tp, :].rearrange("p (g d) -> p g d", d=32)
        a4 = a[:tp, :].rearrange("p (g two d) -> p g two d", two=2, d=32)
        r4 = r[:tp, :].rearrange("p (g two d) -> p g two d", two=2, d=32)
        cosb = ropet[:tp, j, 0:32].unsqueeze(1).to_broadcast([tp, 8, 32])
        sinb = ropet[:tp, j, 32:64].unsqueeze(1).to_broadcast([tp, 8, 32])
        nsinb = ropet[:tp, j, 64:96].unsqueeze(1).to_broadcast([tp, 8, 32])
        TT(r4[:, :, 0, :], a4[:, :, 1, :], nsinb, ALU.mult, R=[a, ropet], W=[r])
        TT(r4[:, :, 1, :], a4[:, :, 0, :], sinb, ALU.mult, R=[a, ropet], W=[r])
        TT(a16, a16, ropet[:tp, j, 0:32].unsqueeze(1).to_broadcast([tp, 16, 32]), ALU.mult, R=[a, ropet], W=[a])
        b_ = qb()
        TT(b_[:tp, :], a[:tp, :], r[:tp, :], ALU.add, R=[a, r], W=[b_])
        bk = gen_banks()
        bkb = bk[:, :].bitcast(BF16)
        for h in range(4):
            TR(bkb[:, h * 128:h * 128 + tp], b_[:tp, h * 128:(h + 1) * 128], ident_b[:tp, :tp], R=[b_, cmb], W=[bk])
        src_ = bkb[:, 0:512].rearrange("p (h t) -> p h t", h=4)
        if dst_T is QT:
            CP(QT[0:64, :, 0, dst_cols], src_[0:64, :, 0:tp], R=[bk], W=[dst_T], eng='act')
            CP(QT[64:128, :, 1, dst_cols], src_[64:128, :, 0:tp], R=[bk], W=[dst_T], eng='dve')
        else:
            CP(dst_T[:, :, dst_cols], src_[:, :, 0:tp], R=[bk], W=[dst_T], eng='act')

    def mark(name):
        P.marks.append((name, len(P.ops)))

    def phaseA(gi):
        meta = gi < 0
        nt = 1 if meta else 4
        tp = 16 if meta else 128
        ntok = nt * tp
        pos0 = 0 if meta else 16 + gi * 512
        tok0 = 0 if meta else gi * 512
        if meta:
            DMA('sp', ropet[:16, 0, :], rope_d[0:16, :], W=[ropet])
        else:
            DMA('sp', ropet[:, :, :], rope_d[pos0:pos0 + 512, :].rearrange("(j p) c -> p j c", p=128), W=[ropet])
        for j in range(nt):
            xt = frow()
            if meta:
                DMA('sp', xt[:16, :], meta_d[:, :], W=[xt])
            else:
                DMA('sp', xt[:, :], x_d[tok0 + j * 128:tok0 + (j + 1) * 128, :], W=[xt])
            s = stat()
            ACT(junk_b[:tp, :], xt[:tp, :], AF.Square, R=[xt], W=[junk_b, s], accum=s[:tp, 0:1])
            rstd_from_ssq(s, 1, 1024, tp)
            h_ = hb()
            TS(h_[:tp, :], xt[:tp, :], s[:tp, 0:1], ALU.mult, R=[xt, s], W=[h_])
            bk = gen_banks()
            bkb = bk[:, :].bitcast(BF16)
            for c in range(8):
                TR(bkb[:, c * 128:c * 128 + tp], h_[:tp, c * 128:(c + 1) * 128], ident_b[:tp, :tp], R=[h_, cmb], W=[bk])
            TT(hT[:, :, j * tp:(j + 1) * tp], bkb[:, :].rearrange("p (c t) -> p c t", c=8)[:, :, 0:tp],
               gmixT[:, :].unsqueeze(2).to_broadcast([128, 8, tp]), ALU.mult, R=[bk, gmixT], W=[hT])

        def tm_block(wt, j, ncols, col0=0):
            w3 = wt[:, :].rearrange("p (c n) -> p c n", c=8)
            bk = gen_banks()
            for c in range(8):
                MM(bk[:tp, 0:ncols], lhsT=hT[:, c, j * tp:(j + 1) * tp], rhs=w3[:, c, col0:col0 + ncols],
                   start=(c == 0), stop=(c == 7), R=[hT, wt], W=[bk])
            return bk

        def fm_chunk(wt, col0, ncol):
            w3 = wt[:, :].rearrange("p (c n) -> p c n", c=8)
            bk = gen_banks()
            for c in range(8):
                MM(bk[:ncol, 0:ntok], lhsT=w3[:, c, col0:col0 + ncol], rhs=hT[:, c, 0:ntok],
                   start=(c == 0), stop=(c == 7), R=[hT, wt], W=[bk])
            return bk

        w0 = w_get()
        bk = fm_chunk(w0, 256, 16)
        CP(gT[:, 0:ntok], bk[:16, 0:ntok], R=[bk], W=[gT], eng='act')
        for j in range(nt):
            bk = gen_banks()
            MM(bk[:tp, 0:256], lhsT=gT[:, j * tp:(j + 1) * tp], rhs=wup_b[:, :], R=[gT, wup_b], W=[bk])
            TT(l_tok[:tp, j, :], bk[:tp, 0:256], vecs[:tp, V_BGATE:V_BGATE + 256], ALU.add, R=[bk, vecs], W=[l_tok])
        ACT(l_tok[:tp, 0:nt, :], l_tok[:tp, 0:nt, :], AF.Exp, R=[l_tok], W=[l_tok], scale=-1.0)
        ACT(l_tok[:tp, 0:nt, :], l_tok[:tp, 0:nt, :], AF.Ln, R=[l_tok], W=[l_tok], bias=1.0)
        for j in range(nt):
            bk = gen_banks()
            MM(bk[:tp, 0:256], lhsT=sgtS_f[:tp, :tp], rhs=l_tok[:tp, j, :], R=[cm, l_tok], W=[bk])
            e_ = ek()
            ACT(e_[:tp, :], bk[:tp, 0:256], AF.Exp, R=[bk], W=[e_])
            bk2 = tm_block(w0, j, 256, 0)
            TT(khat[:tp, j, :], bk2[:tp, 0:256], e_[:tp, :], ALU.mult, R=[bk2, e_], W=[khat])
            if not meta:
                bk3 = gen_banks()
                for p in range(2):
                    MM(bk3[:, p * 128:p * 128 + tp], lhsT=l_tok[:tp, j, p * 128:(p + 1) * 128], rhs=triS_f[:tp, :tp],
                       R=[l_tok, cm], W=[bk3])
                v3 = bk3[:, 0:256].rearrange("p (a t) -> p a t", a=2)
                ACT(ebT[:, :, j * 128:(j + 1) * 128], v3, AF.Exp, R=[bk3], W=[ebT])
                ACT(enbT[:, :, j * 128:(j + 1) * 128], v3, AF.Exp, R=[bk3], W=[enbT], scale=-1.0)
                ACT(eblast[:, :, j:j + 1], v3[:, :, 127:128], AF.Exp, R=[bk3], W=[eblast])
        w_done()
        if not meta:
            w1 = w_get()
            for p in range(2):
                bk = fm_chunk(w1, p * 128, 128)
                STT(qtT[:, p, :], bk[:, 0:512], 0.125, ebT[:, p, :], ALU.mult, ALU.mult, R=[bk, ebT], W=[qtT])
            for p in range(2):
                bk = fm_chunk(w1, 256 + p * 128, 128)
                TT(ktT[:, p, :], bk[:, 0:512], enbT[:, p, :], ALU.mult, R=[bk, enbT], W=[ktT])
            w_done()
        wt = w_get()
        for j in range(nt):
            bk = tm_block(wt, j, 512)
            qk_norm_rope(bk, tp, j, V_GK, KT[gi], slice(j * tp, (j + 1) * tp))
        w_done()
        wt = w_get()
        for j in range(nt):
            bk = tm_block(wt, j, 512)
            CP(VA[gi][:tp, j, :, 0:128], bk[:tp, :].rearrange("p (h e) -> p h e", h=4), R=[bk], W=[VA[gi]], eng='act')
        w_done()
        if not meta:
            MEMSET(scr8[:, :], 0.0, W=[QT])
            wt = w_get()
            for j in range(nt):
                bk = tm_block(wt, j, 512)
                qk_norm_rope(bk, tp, j, V_GQ, QT, slice(j * 128, (j + 1) * 128))
            w_done()
        wt = w_get()
        for j in range(nt):
            bk = tm_block(wt, j, 512)
            CP(vg[:tp, j, :], bk[:tp, :], R=[bk], W=[vg], eng='act')
        w_done()
        if not meta:
            wt = w_get()
            for j in range(nt):
                bk = tm_block(wt, j, 512)
                ACT(sr[:, j, :], bk[:, :], AF.Silu, R=[bk], W=[sr])
            w_done()

        mark(f'g{gi}.inproj')
        if gi == 0:
            zsrc = base_oT[:, :].bitcast(BF16)
            DMA('sp', Yg_d[NSLOT:NSLOT + 128, :], base_oT[:, 0:1024], R=oT_tok[0] + oT_tok[1], W=[Yg_d])
            for zb in range(NSLOT // 512):
                DMA('sp', Xg_d[zb * 512:(zb + 1) * 512, :].rearrange("(p r) d -> p (r d)", r=4), zsrc,
                    R=oT_tok[0] + oT_tok[1], W=[Xg_d])
        if not meta:
            ktiles = [(-1, 0, 16)]
            for g2 in range(gi):
                for j2 in range(4):
                    ktiles.append((g2, j2, 128))
            items = []
            for h in range(4):
                for (g2, j2, nk) in ktiles + [(gi, r, 128) for r in range(4)]:
                    for m in range(2):
                        items.append(dict(h=h, g2=g2, j2=j2, nk=nk, m=m))

            def emit_S(it):
                h, g2, j2, nk, m = it['h'], it['g2'], it['j2'], it['nk'], it['m']
                diag = (g2 == gi)
                jq0 = j2 if diag else 0
                nq = 512 - jq0 * 128
                kT = KT[g2][:, h, j2 * nk:(j2 + 1) * nk] if g2 >= 0 else KT[-1][:, h, 0:16]
                bk = gen_banks()
                if diag:
                    MM(bk[:, 0:128], lhsT=ident_b, rhs=maskb_b, start=True, stop=True, R=[cmb], W=[bk])
                    MM(bk[:, 0:nq], lhsT=kT, rhs=QT[:, h, m, jq0 * 128:512], start=False, stop=True, sgc=True,
                       R=[KT[g2], QT], W=[bk])
                else:
                    MM(bk[:nk, 0:512], lhsT=kT, rhs=QT[:, h, m, 0:512], R=[KT[g2], QT], W=[bk])
                pt = PT()
                ACT(pt[:nk, 0:nq], bk[:nk, 0:nq], AF.Exp, R=[bk], W=[pt], scale=0.125)
                it['pt'] = pt
                it['jq0'] = jq0

            def emit_PV(it):
                h, g2, j2, nk, m = it['h'], it['g2'], it['j2'], it['nk'], it['m']
                diag = (g2 == gi)
                jq0 = it['jq0']
                pt = it['pt']
                if g2 == -1 and m == 0:
                    for bi in range(4):
                        MEMSET(banks[4 + bi][:, 0:258], 0.0, W=[banks[4 + bi]])
                for jq in range(jq0, 4):
                    MM(acc_tile[(m, jq)], lhsT=pt[:nk, (jq - jq0) * 128:(jq - jq0 + 1) * 128],
                       rhs=VA[g2][:nk, j2, h, :], start=False, stop=False, sgc=True,
                       R=[pt, VA[g2]], W=[acc_tok[(m, jq)]])
                if diag and m == 1:
                    jq = j2
                    a0, a1 = acc_tile[(0, jq)], acc_tile[(1, jq)]
                    t0 = acc_tok[(0, jq)]
                    s = stat()
                    RECIP(s[:, 0:1], a0[:, 128:129], R=[t0], W=[s])
                    RECIP(s[:, 1:2], a1[:, 128:129], R=[t0], W=[s])
                    TT(s[:, 2:3], s[:, 1:2], neglam[:, :], ALU.mult, R=[s, neglam], W=[s])
                    o1 = o_sb()
                    TS(o1[:, :], a1[:, 0:128], s[:, 2:3], ALU.mult, R=[t0, s], W=[o1])
                    o = o_sb()
                    s2 = stat()
                    STT(o[:, :], a0[:, 0:128], s[:, 0:1], o1[:, :], ALU.mult, ALU.add, R=[t0, s, o1], W=[o])
                    STT(junk_d2[:, :], o[:, :], 1.0, o[:, :], ALU.mult, ALU.mult, R=[o], W=[junk_d2, s2], accum=s2[:, 0:1])
                    rstd_from_ssq(s2, 1, 128, 128)
                    STT(oa_tok[jq][:, h * 128:(h + 1) * 128], o[:, :], s2[:, 0:1], gsub_s[:, :], ALU.mult, ALU.mult,
                        R=[o, s2, gsub_s], W=[oa_tok[jq]])

            LA = 2
            for i in range(len(items) + LA):
                if i < len(items):
                    emit_S(items[i])
                if i >= LA:
                    emit_PV(items[i - LA])
            for jq in range(4):
                if dbg:
                    f = frow()
                    CP(f[:, 0:512], oa_tok[jq][:, :], R=[oa_tok[jq]], W=[f])
                    DMA('sp', dbg_d["d_oa"][tok0 + jq * 128:tok0 + (jq + 1) * 128, :], f[:, 0:512], R=[f], W=[dbg_d["d_oa"]])
                bk = gen_banks()
                bkb = bk[:, :].bitcast(BF16)
                for h in range(4):
                    TR(bkb[:, h * 128:(h + 1) * 128], oa_tok[jq][:, h * 128:(h + 1) * 128], ident_b, R=[oa_tok[jq], cmb], W=[bk])
                CP(oT[:, 0, :, jq * 128:(jq + 1) * 128], bkb[:, 0:512].rearrange("p (h t) -> p h t", h=4),
                   R=[bk], W=[oT_tok[0][jq]], eng='act')

        mark(f'g{gi}.attn')
        for j in range(nt):
            cols = slice(j * tp, (j + 1) * tp)
            ob = ob_tok() if not meta else None
            for h in range(4):
                p, hh = h // 2, h % 2
                rows = slice(hh * 64, (hh + 1) * 64)
                if not meta:
                    bk = gen_banks()
                    MM(bk[:, 0:128], lhsT=ktT[rows, p, cols], rhs=qtT[rows, p, cols], R=[ktT, qtT], W=[bk])
                    at = AT()
                    TT(at[:, :], bk[:, 0:128], tri01_f, ALU.mult, R=[bk, cm], W=[at])
                    bo = gen_banks()
                    MM(bo[:, 0:128], lhsT=at[:, :], rhs=vg[:, j, h * 128:(h + 1) * 128], start=True, stop=False, R=[at, vg], W=[bo])
                    MM(bo[:, 0:128], lhsT=qtT[rows, p, cols], rhs=state_b[rows, p, :], start=False, stop=True,
                       R=[qtT, state_b], W=[bo])
                bkv = gen_banks()
                MM(bkv[:, 0:128], lhsT=khat[:tp, j, p * 128:(p + 1) * 128], rhs=vg[:tp, j, h * 128:(h + 1) * 128],
                   R=[khat, vg], W=[bkv])
                if meta:
                    CP(state_f[rows, p, :], bkv[rows, 0:128], R=[bkv], W=[state_f])
                else:
                    STT(state_f[rows, p, :], state_f[rows, p, :], eblast[rows, p, j:j + 1], bkv[rows, 0:128], ALU.mult, ALU.add,
                        R=[state_f, eblast, bkv], W=[state_f])
                CP(state_b[rows, p, :], state_f[rows, p, :], R=[state_f], W=[state_b], eng='act')
                if not meta:
                    s2 = stat()
                    ACT(junk_f[:, 0:128], bo[:, 0:128], AF.Square, R=[bo], W=[junk_f, s2], accum=s2[:, 0:1])
                    rstd_from_ssq(s2, 1, 128, 128)
                    o = o_sb()
                    STT(o[:, :], bo[:, 0:128], s2[:, 0:1], vecs[:, V_GGLA:V_GGLA + 128], ALU.mult, ALU.mult, R=[bo, s2, vecs], W=[o])
                    TT(ob[:, h * 128:(h + 1) * 128], o[:, :], sr[:, j, h * 128:(h + 1) * 128], ALU.mult, R=[o, sr], W=[ob])
            if not meta:
                if dbg:
                    f = frow()
                    CP(f[:, 0:512], ob[:, :], R=[ob], W=[f])
                    DMA('sp', dbg_d["d_ob"][tok0 + j * 128:tok0 + (j + 1) * 128, :], f[:, 0:512], R=[f], W=[dbg_d["d_ob"]])
                bk = gen_banks()
                bkb = bk[:, :].bitcast(BF16)
                for h in range(4):
                    TR(bkb[:, h * 128:(h + 1) * 128], ob[:, h * 128:(h + 1) * 128], ident_b, R=[ob, cmb], W=[bk])
                CP(oT[:, 1, :, j * 128:(j + 1) * 128], bkb[:, 0:512].rearrange("p (h t) -> p h t", h=4),
                   R=[bk], W=[oT_tok[1][j]], eng='act')
        if meta:
            return

        mark(f'g{gi}.gla')
        for half2 in range(2):
            BR, G_a, G_b = w_get(0), w_get(1), w_get(2)
            BR5 = BR[:, :].rearrange("p (gbl n wc col) -> p gbl n wc col", gbl=2, n=2, wc=4)
            for gbl, G in ((0, G_a), (1, G_b)):
                gb = half2 * 2 + gbl
                G3 = G[:, :].rearrange("p (c n) -> p c n", c=8)
                for cl in range(2):
                    c = 2 * gb + cl
                    parts = []
                    for n_ in range(2):
                        bg = gen_banks()
                        for cc in range(8):
                            MM(bg[:, :], lhsT=G3[:, cc, n_ * 256 + cl * 128:n_ * 256 + (cl + 1) * 128], rhs=hT[:, cc, :],
                               start=(cc == 0), stop=(cc == 7), R=[G, hT], W=[bg])
                        s_ = m0()
                        ACT(s_[:, :], bg[:, :], AF.Sigmoid, R=[bg, bmgT], W=[s_], bias=bmgT[:, n_ * 8 + c:n_ * 8 + c + 1])
                        by = gen_banks()
                        for wc in range(4):
                            MM(by[:, :], lhsT=BR5[:, gbl, n_, wc, cl * 128:(cl + 1) * 128], rhs=oT[:, n_, wc, :],
                               start=(wc == 0), stop=(wc == 3), R=[BR] + oT_tok[n_], W=[by])
                        TT(s_[:, :], by[:, :], s_[:, :], ALU.mult, R=[by, s_], W=[s_])
                        parts.append(s_)
                    m_, m2 = parts
                    TT(mT[:, c, :], m_[:, :], m2[:, :], ALU.add, R=[m_, m2], W=[mT], eng='pool')
            w_done(3)

        mark(f'g{gi}.merge')
        O0, O1 = w_get(0), w_get(1)
        O3 = [O0[:, :].rearrange("p (c n) -> p c n", c=8), O1[:, :].rearrange("p (c n) -> p c n", c=8)]
        Ot = [O0, O1]
        for j in range(4):
            gj = gi * 4 + j
            rows_d = slice(tok0 + j * 128, tok0 + (j + 1) * 128)
            xt = frow()
            DMA('sp', xt[:, :], x_d[rows_d, :], W=[xt])
            u = frow()
            for half in range(2):
                bk = gen_banks()
                for c in range(8):
                    MM(bk[:, :], lhsT=mT[:, c, j * 128:(j + 1) * 128], rhs=O3[half][:, c, :], start=(c == 0), stop=(c == 7),
                       R=[mT, Ot[half]], W=[bk])
                TT(u[:, half * 512:(half + 1) * 512], bk[:, :], xt[:, half * 512:(half + 1) * 512], ALU.add, R=[bk, xt], W=[u])
            DMA('sp', U_d[rows_d, :], u[:, :], R=[u], W=[U_d])
            if dbg:
                DMA('sp', dbg_d["d_u"][rows_d, :], u[:, :], R=[u], W=[dbg_d["d_u"]])
            s = stat()
            ACT(junk_b[:, :], u[:, :], AF.Square, R=[u], W=[junk_b, s], accum=s[:, 0:1])
            rstd_from_ssq(s, 1, 1024, 128)
            h2f = frow()
            STT(h2f[:, :], u[:, :], s[:, 0:1], vecs[:, V_GFFN:V_GFFN + 1024], ALU.mult, ALU.mult, R=[u, s, vecs], W=[h2f])
            h2 = h2b()
            CP(h2[:, :], h2f[:, :], R=[h2f], W=[h2], eng='act')
            for q4 in range(2):
                bk = gen_banks()
                for c4 in range(4):
                    c = q4 * 4 + c4
                    TR(bk[:, c4 * 128:(c4 + 1) * 128], h2f[:, c * 128:(c + 1) * 128], ident_f, R=[h2f, cm], W=[bk])
                CP(h2T[:, q4 * 4:(q4 + 1) * 4, :], bk[:, :].rearrange("p (c t) -> p c t", c=4), R=[bk], W=[h2T],
                   eng=('act' if q4 == 0 else 'dve'))
            bl = gen_banks()
            wr3 = wr_sb[:, :].rearrange("p (c n) -> p c n", c=8)
            for c in range(8):
                MM(bl[:, 0:36], lhsT=h2T[:, c, :], rhs=wr3[:, c, :], start=(c == 0), stop=(c == 7), R=[h2T, wr_sb], W=[bl])
            lg = s36()
            TT(lg[:, :], bl[:, 0:36], vecs[:, V_BR:V_BR + 36], ALU.add, R=[bl, vecs], W=[lg])
            if dbg:
                DMA('sp', dbg_d["d_lg"][rows_d, :], lg[:, :], R=[lg], W=[dbg_d["d_lg"]])
            s = stat()
            RED(s[:, 0:1], lg[:, 0:4], ALU.max, R=[lg], W=[s])
            gone = stat()
            TS(gone[:, 0:4], lg[:, 0:4], s[:, 0:1], ALU.is_equal, R=[lg, s], W=[gone])
            TS(s[:, 1:2], s[:, 0:1], -1.0, ALU.mult, R=[s], W=[s])
            ACT(junk_f[:, 0:4], lg[:, 0:4], AF.Exp, R=[lg, s], W=[junk_f, s], bias=s[:, 1:2], accum=s[:, 2:3])
            RECIP(s[:, 3:4], s[:, 2:3], R=[s], W=[s])
            t36 = s36()
            TT(t36[:, 0:32].rearrange("p (g e) -> p g e", g=4), lg[:, 4:36].rearrange("p (g e) -> p g e", g=4),
               gone[:, 0:4].unsqueeze(2).to_broadcast([128, 4, 8]), ALU.mult, R=[lg, gone], W=[t36])
            es_ = stat()
            RED(es_[:, 0:8], t36[:, 0:32].rearrange("p (g e) -> p e g", g=4), ALU.add, R=[t36], W=[es_])
            RED(s[:, 4:5], es_[:, 0:8], ALU.max, R=[es_], W=[s])
            one1 = stat()
            TS(one1[:, 0:8], es_[:, 0:8], s[:, 4:5], ALU.is_equal, R=[es_, s], W=[one1])
            es2 = stat()
            STT(es2[:, 0:8], one1[:, 0:8], -1e30, es_[:, 0:8], ALU.mult, ALU.add, R=[one1, es_], W=[es2])
            RED(s[:, 5:6], es2[:, 0:8], ALU.max, R=[es2], W=[s])
            one2 = stat()
            TS(one2[:, 0:8], es2[:, 0:8], s[:, 5:6], ALU.is_equal, R=[es2, s], W=[one2])
            TT(s[:, 6:7], s[:, 5:6], s[:, 4:5], ALU.subtract, R=[s], W=[s])
            ACT(s[:, 7:8], s[:, 6:7], AF.Exp, R=[s], W=[s])
            w_ = stat()
            TS(w_[:, 0:1], s[:, 7:8], 1.0, ALU.add, R=[s], W=[w_])
            RECIP(w_[:, 1:2], w_[:, 0:1], R=[w_], W=[w_])
            TT(w_[:, 2:3], s[:, 7:8], w_[:, 1:2], ALU.mult, R=[s, w_], W=[w_])
            TS(cw_all[:, gj, :], w_[:, 1:3], s[:, 3:4], ALU.mult, R=[w_, s], W=[cw_all])
            M1 = s36()
            TT(M1[:, 0:32].rearrange("p (g e) -> p g e", g=4), gone[:, 0:4].unsqueeze(2).to_broadcast([128, 4, 8]),
               one1[:, 0:8].unsqueeze(1).to_broadcast([128, 4, 8]), ALU.mult, R=[gone, one1], W=[M1])
            M2 = s36()
            TT(M2[:, 0:32].rearrange("p (g e) -> p g e", g=4), gone[:, 0:4].unsqueeze(2).to_broadcast([128, 4, 8]),
               one2[:, 0:8].unsqueeze(1).to_broadcast([128, 4, 8]), ALU.mult, R=[gone, one2], W=[M2])
            Mt = s36()
            TT(Mt[:, 0:32], M1[:, 0:32], M2[:, 0:32], ALU.add, R=[M1, M2], W=[Mt])
            bp = gen_banks()
            MM(bp[:, 0:32], lhsT=slt01_f, rhs=Mt[:, 0:32], R=[cm, Mt], W=[bp])
            MM(bp[:, 32:64], lhsT=ones_f[:, :], rhs=Mt[:, 0:32], R=[ones_f, Mt], W=[bp])
            posb = s36()
            TT(posb[:, 0:32], bp[:, 0:32], cntb[:, :], ALU.add, R=[bp, cntb], W=[posb])
            STT(junk_d[:, 0:32], posb[:, 0:32], 1.0, M1[:, 0:32], ALU.mult, ALU.mult, R=[posb, M1], W=[junk_d, slots_f],
                accum=slots_f[:, gj, 0:1])
            STT(junk_d[:, 32:64], posb[:, 0:32], 1.0, M2[:, 0:32], ALU.mult, ALU.mult, R=[posb, M2], W=[junk_d, slots_f],
                accum=slots_f[:, gj, 1:2])
            TT(cntb[:, :], cntb[:, :], bp[:, 32:64], ALU.add, R=[cntb, bp], W=[cntb])
            okm = s36()
            TT(okm[:, 0:32], posb[:, 0:32], limb[:, :], ALU.is_lt, R=[posb, limb], W=[okm])
            ok2 = stat()
            for k_, Mk in ((0, M1), (1, M2)):
                STT(junk_d[:, 0:32], okm[:, 0:32], 1.0, Mk[:, 0:32], ALU.mult, ALU.mult, R=[okm, Mk], W=[junk_d, ok2],
                    accum=ok2[:, k_:k_ + 1])
                TS(ok2[:, 2 + k_:3 + k_], slots_f[:, gj, k_:k_ + 1], -float(NSLOT), ALU.add, R=[slots_f, ok2], W=[ok2])
                STT(slots_f[:, gj, k_:k_ + 1], ok2[:, 2 + k_:3 + k_], ok2[:, k_:k_ + 1], trashc[:, :], ALU.mult, ALU.add,
                    R=[ok2, trashc], W=[slots_f])
            TT(cw_all[:, gj, :], cw_all[:, gj, :], ok2[:, 0:2], ALU.mult, R=[cw_all, ok2], W=[cw_all])
            CP(slots_i[:, gj, :], slots_f[:, gj, :], R=[slots_f], W=[slots_i])
            if dbg:
                f = stat()
                CP(f[:, 0:2], slots_f[:, gj, :], R=[slots_f], W=[f])
                CP(f[:, 2:4], cw_all[:, gj, :], R=[cw_all], W=[f])
                DMA('sp', dbg_d["d_sl"][rows_d, :], f[:, 0:4], R=[f], W=[dbg_d["d_sl"]])
            for k_ in range(2):
                P.dma('pool', lambda e, gj=gj, k_=k_, h2=h2: e.indirect_dma_start(
                    out=Xg_d[:, :], out_offset=bass.IndirectOffsetOnAxis(ap=slots_i[:, gj, k_:k_ + 1], axis=0),
                    in_=h2[:, :], in_offset=None), _toks([h2, slots_i]), _toks([Xg_d]), nbytes=262144)
        w_done(2)

    phaseA(-1)
    for gi in range(NG):
        phaseA(gi)

    mark('A7.last')
    o_ = 0
    XgT2, xg2 = [], []
    for i in range(2):
        XgT2.append(Tile(arena[:, o_:o_ + 8 * CAP].rearrange("p (c t) -> p c t", c=8), f"XgT{i}"))
        o_ += 8 * CAP
        xg2.append(Tile(arena[:, o_:o_ + NB * 1024].rearrange("p (b d) -> p b d", b=NB), f"xg{i}"))
        o_ += NB * 1024
    hidT = Tile(arena[:, o_:o_ + 4 * CAP].rearrange("p (c t) -> p c t", c=4), "hidT")
    o_ += 4 * CAP
    wbB = list(wbufs)
    while o_ + 4096 <= AR_KT + AR_VA and len(wbB) < 9:
        wbB.append(Tile(arena[:, o_:o_ + 4096], f"wbB{len(wbB)}"))
        o_ += 4096
    assert o_ <= AR_KT + AR_VA
    NWB2 = len(wbB)
    bar = P.sb([128, 1], F32, "bar")
    P.op('dve', lambda e: e.memset(bar[:, :], 0.0), arena_toks,
         _toks(XgT2 + xg2 + [hidT, bar] + wbB[NWB:]))
    wsB = dict(load=0, use=0)
    gen_banks = Rot(banks[0:8])

    mT_toks = QT.toks
    stg = [
        [(hT.t[:, :, :].rearrange("p c t -> p (c t)").bitcast(F32), hT.toks, 0, 2048, 'dve'),
         (scr8[:, :].bitcast(F32), mT_toks, 2048, 4096, 'act')],
        [(base_oT[:, :], oT_tok[0] + oT_tok[1], 0, 2048, 'dve'),
         (base_sr[:, :], sr.toks, 2048, 3072, 'act'),
         (base_vg[:, :], vg.toks, 3072, 4096, 'act')],
    ]
    hw = dict(n=0)

    def wB_issue():
        k = wsB['load']
        if k < 96:
            buf = wbB[k % NWB2]
            if k % 5 in (1, 3):
                for (ap_, toks_, c0, c1, eng_) in stg[hw['n'] % 2]:
                    DMA('sp', ap_, wE_d[k][:, c0:c1], R=[], W=toks_)
                    CP(buf[:, c0:c1], ap_, R=toks_, W=[buf], eng=eng_)
                hw['n'] += 1
            else:
                DMA('pool', buf[:, :], wE_d[k], R=[], W=[buf])
            wsB['load'] += 1

    for _ in range(NWB2):
        wB_issue()

    def prep(e_):
        xg, XgT = xg2[e_ % 2], XgT2[e_ % 2]
        DMA('sp', xg[:, :, :], Xg_d[e_ * CAP:(e_ + 1) * CAP, :].rearrange("(b p) d -> p b d", p=128), R=[Xg_d], W=[xg])
        for b_ in range(NB):
            bk = gen_banks()
            bkb = bk[:, :].bitcast(BF16)
            for c in range(8):
                TR(bkb[:, c * 128:(c + 1) * 128], xg[:, b_, c * 128:(c + 1) * 128], ident_b, R=[xg, cmb], W=[bk])
            CP(XgT[:, :, b_ * 128:(b_ + 1) * 128], bkb[:, :].rearrange("p (c t) -> p c t", c=8), R=[bk], W=[XgT],
               eng=('act' if b_ % 2 == 0 else 'dve'))

    prep(0)
    for e_ in range(32):
        if e_ + 1 < 32:
            prep(e_ + 1)
        XgT = XgT2[e_ % 2]
        wg, wu, wd = (wbB[(wsB['use'] + i) % NWB2] for i in range(3))
        wg3 = wg[:, :].rearrange("p (c n) -> p c n", c=8)
        wu3 = wu[:, :].rearrange("p (c n) -> p c n", c=8)
        wd3 = wd[:, :].rearrange("p (c n) -> p c n", c=4)
        nch = -(-CAP // 512)
        csz = CAP // nch
        assert csz * nch == CAP
        for fc in range(4):
            for ch in range(nch):
                tsl = slice(ch * csz, (ch + 1) * csz)
                bg = gen_banks()
                for c in range(8):
                    MM(bg[:, 0:csz], lhsT=wg3[:, c, fc * 128:(fc + 1) * 128], rhs=XgT[:, c, tsl], start=(c == 0), stop=(c == 7),
                       R=[wg, XgT], W=[bg])
                bu = gen_banks()
                for c in range(8):
                    MM(bu[:, 0:csz], lhsT=wu3[:, c, fc * 128:(fc + 1) * 128], rhs=XgT[:, c, tsl], start=(c == 0), stop=(c == 7),
                       R=[wu, XgT], W=[bu])
                s_ = frow()
                ACT(s_[:, 0:csz], bg[:, 0:csz], AF.Silu, R=[bg], W=[s_])
                TT(hidT[:, fc, tsl], bu[:, 0:csz], s_[:, 0:csz], ALU.mult, R=[bu, s_], W=[hidT])
        for b_ in range(NB):
            y = frow()
            for half in range(2):
                bk = gen_banks()
                for fc in range(4):
                    MM(bk[:, :], lhsT=hidT[:, fc, b_ * 128:(b_ + 1) * 128], rhs=wd3[:, fc, half * 512:(half + 1) * 512],
                       start=(fc == 0), stop=(fc == 3), R=[hidT, wd], W=[bk])
                CP(y[:, half * 512:(half + 1) * 512], bk[:, :], R=[bk], W=[y], eng=('act' if half == 0 else 'dve'))
            DMA('sp', Yg_d[e_ * CAP + b_ * 128:e_ * CAP + (b_ + 1) * 128, :], y[:, :], R=[y], W=[Yg_d])
        for _ in range(3):
            wsB['use'] += 1
            wB_issue()

    mark('experts')
    for gj in range(NT):
        rows_d = slice(gj * 128, (gj + 1) * 128)
        y1, y2, u = frow(), frow(), frow()
        for k_, yt in ((0, y1), (1, y2)):
            P.dma('pool', lambda e, gj=gj, k_=k_, yt=yt: e.indirect_dma_start(
                out=yt[:, :], out_offset=None, in_=Yg_d[:, :],
                in_offset=bass.IndirectOffsetOnAxis(ap=slots_i[:, gj, k_:k_ + 1], axis=0)),
                _toks([Yg_d, slots_i]), _toks([yt]), nbytes=524288)
        DMA('sp', u[:, :], U_d[rows_d, :], R=[U_d], W=[u])
        STT(u[:, :], y1[:, :], cw_all[:, gj, 0:1], u[:, :], ALU.mult, ALU.add, R=[y1, cw_all, u], W=[u])
        STT(u[:, :], y2[:, :], cw_all[:, gj, 1:2], u[:, :], ALU.mult, ALU.add, R=[y2, cw_all, u], W=[u])
        DMA('sp', out_d[rows_d, :], u[:, :], R=[u], W=[out_d])

    mark('combine')
    P.build()
    return nc, P.stats


def host_prep(inp, T):
    f = lambda a: np.ascontiguousarray(np.asarray(a, dtype=np.float32))
    w_in = f(inp['w_in'])[0]
    o = np.cumsum([0, 512, 512, 512, 256, 256, 512, 512, 16, 2048])
    da_q, da_k, da_v = w_in[:, o[0]:o[1]], w_in[:, o[1]:o[2]], w_in[:, o[2]:o[3]]
    gq, gk, gv, gr, gg = (w_in[:, o[3]:o[4]], w_in[:, o[4]:o[5]], w_in[:, o[5]:o[6]], w_in[:, o[6]:o[7]], w_in[:, o[7]:o[8]])
    gates = w_in[:, o[8]:o[9]]
    z = np.zeros((1024, 512 - 272), np.float32)
    blocks = [np.concatenate([gk, gg, z], 1), np.concatenate([gq, gk], 1), da_k, da_v, da_q, gv, gr]
    for gb in range(4):
        blocks.append(np.concatenate([gates[:, gb * 256:(gb + 1) * 256], gates[:, 1024 + gb * 256:1024 + (gb + 1) * 256]], 1))

    def blk8(w):
        return w.reshape(8, 128, 512).transpose(1, 0, 2).reshape(128, 4096)

    def blk4(w):
        return w.reshape(4, 128, 1024).transpose(1, 0, 2).reshape(128, 4096)
    wA = [blk8(b) for b in blocks]
    wbr = f(inp['w_branch'])[0]
    brp = wbr.reshape(2, 4, 128, 4, 256).transpose(3, 2, 0, 1, 4).reshape(2, 2, 128, 2048).transpose(0, 2, 1, 3).reshape(2, 128, 4096)
    wA += [brp[0], brp[1]]
    wo = f(inp['w_out'])[0]
    wA += [blk8(wo[:, 0:512]), blk8(wo[:, 512:1024])]
    wA = np.ascontiguousarray(np.stack(wA, 0))
    weg, weu, wed = f(inp['w_exp_gate'])[0], f(inp['w_exp_up'])[0], f(inp['w_exp_down'])[0]
    wE = np.empty((96, 128, 4096), np.float32)
    wE[0::3] = weg.reshape(32, 8, 128, 512).transpose(0, 2, 1, 3).reshape(32, 128, 4096)
    wE[1::3] = weu.reshape(32, 8, 128, 512).transpose(0, 2, 1, 3).reshape(32, 128, 4096)
    wE[2::3] = wed.reshape(32, 4, 128, 1024).transpose(0, 2, 1, 3).reshape(32, 128, 4096)
    wr = np.concatenate([f(inp['w_router_group'])[0], f(inp['w_router_expert'])[0].reshape(1024, 32)], 1)
    wr = np.ascontiguousarray(wr.reshape(8, 128, 36).transpose(1, 0, 2).reshape(128, 288))
    pos = np.arange(T + 16, dtype=np.float32)
    inv = np.power(np.float32(10000.0), -np.arange(32, dtype=np.float32) * 2.0 / 64).astype(np.float32)
    ang = pos[:, None] * inv[None, :]
    rope = np.concatenate([np.cos(ang), np.sin(ang), -np.sin(ang)], 1).astype(np.float32)
    s = np.arange(128)[:, None]
    t = np.arange(128)[None, :]
    cmask = np.concatenate([(s == t), (s <= t), (s < t), np.where(s <= t, 0.0, -30000.0),
                            (s <= t) * (-1.0 / 16), (s > t) * (-1.0 / 16)], 1).astype(np.float32)
    gmixT = np.ascontiguousarray(f(inp['g_mix_norm'])[0].reshape(8, 128).T)
    bmgT = np.ascontiguousarray(f(inp['b_merge_gate'])[0].reshape(16, 128).T)
    vecs = np.zeros((1, 1956), np.float32)
    vecs[0, 0:1024] = f(inp['g_ffn_norm'])[0]
    vecs[0, 1024:1088] = f(inp['g_q_norm'])[0]
    vecs[0, 1088:1152] = f(inp['g_k_norm'])[0]
    vecs[0, 1152:1280] = f(inp['g_diff_subln'])[0]
    vecs[0, 1280:1408] = f(inp['g_gla_norm'])[0]
    vecs[0, 1408:1664] = f(inp['b_gla_gate'])[0]
    vecs[0, 1664:1668] = f(inp['b_router_group'])[0]
    vecs[0, 1668:1700] = f(inp['b_router_expert'])[0].reshape(32)
    vecs[0, 1700:1764] = f(inp['lambda_q1'])[0]
    vecs[0, 1764:1828] = f(inp['lambda_k1'])[0]
    vecs[0, 1828:1892] = f(inp['lambda_q2'])[0]
    vecs[0, 1892:1956] = f(inp['lambda_k2'])[0]
    shared = dict(meta=f(inp['meta_tokens']), wA=wA, wE=wE, wr=wr, rope=rope, cmask=cmask, gmixT=gmixT, bmgT=bmgT,
                  vecs=vecs, wup=f(inp['w_gla_gate_up'])[0])
    return shared


_CACHE = {}


def run(inp, T, CAP, ncores, dbg=False):
    key = (T, CAP, dbg)
    if key not in _CACHE:
        _CACHE[key] = build_program(T, CAP, dbg)
    nc, stats = _CACHE[key]
    shared = host_prep(inp, T)
    shared['ebase'] = (np.arange(32, dtype=np.float32) * CAP).reshape(1, 32)
    x = np.asarray(inp['x'], dtype=np.float32)
    in_maps = []
    for b in range(ncores):
        d = dict(shared)
        d['x'] = np.ascontiguousarray(x[b])
        in_maps.append(d)
    res = run_bass_kernel_spmd(nc, in_maps, core_ids=list(range(ncores)))
    return res, stats


def kernel(**inputs):
    res, _ = run(inputs, 4096, 512, 8)
    return np.stack([np.asarray(r["out"], dtype=np.float32) for r in res.results], 0)
```
